# Optimizing a Trainium2 kernel written in Bass

```python
import jax, jax.numpy as jnp
from jax import lax
import numpy as np

D_MODEL = 1024
BATCH = 16
SEQ = 2048
DEPTH = 1

HEAD_DIM = 64
N_SLOTS = 8
DILATED_GROUPS = ((128, 1), (512, 4), (2048, 16))
N_ATTN_HEADS = N_SLOTS * len(DILATED_GROUPS)
ATTN_WIDTH = N_ATTN_HEADS * HEAD_DIM
ATTN_OUT_WIDTH = N_SLOTS * HEAD_DIM
LRU_WIDTH = D_MODEL
LRU_BLOCKS = 16
LRU_BLOCK_DIM = LRU_WIDTH // LRU_BLOCKS
CONV_WIDTH = 4
LRU_C = 8.0
N_BRANCHES = 2
IN_COLS = 3 * ATTN_WIDTH + 2 * LRU_WIDTH + N_BRANCHES * D_MODEL
IN_SPLITS = [ATTN_WIDTH, 2 * ATTN_WIDTH, 3 * ATTN_WIDTH,
             3 * ATTN_WIDTH + LRU_WIDTH, 3 * ATTN_WIDTH + 2 * LRU_WIDTH]
N_EXPERT_GROUPS = 4
EXPERTS_PER_GROUP = 8
N_EXPERTS = N_EXPERT_GROUPS * EXPERTS_PER_GROUP
TOP_K_INNER = 2
D_EXPERT = D_MODEL // 2
MOE_BLOCK = 128
EPS = 1e-6

kernel_name = "hybrid_dilated_attn_rglru_hmoe_adaln"


def rms_norm(x, g):
    xf = x.astype(jnp.float32)
    y = xf * lax.rsqrt(jnp.mean(xf * xf, axis=-1, keepdims=True) + EPS)
    return (y * g.astype(jnp.float32)).astype(x.dtype)


def dilated_window_group(q, k, v, window, dilation):
    b, s, h, dh = q.shape
    span = window // dilation
    n_sub = s // dilation
    n_blk = -(-n_sub // span)
    pad = n_blk * span - n_sub

    def to_blocks(t):
        t = t.reshape(b, n_sub, dilation, h, dh)
        t = jnp.pad(t, ((0, 0), (0, pad), (0, 0), (0, 0), (0, 0)))
        return t.reshape(b, n_blk, span, dilation, h, dh)

    def with_prev(t):
        prev = jnp.pad(t, ((0, 0), (1, 0), (0, 0), (0, 0), (0, 0), (0, 0)))[:, :-1]
        return jnp.concatenate([prev, t], axis=2)

    qb = to_blocks(q)
    kk = with_prev(to_blocks(k))
    vv = with_prev(to_blocks(v))
    scores = jnp.einsum('bnqphd,bnkphd->bnqphk', qb, kk,
                        preferred_element_type=jnp.float32) * (dh ** -0.5)
    qi = jnp.arange(span)[:, None]
    ki = jnp.arange(2 * span)[None, :]
    dist = span + qi - ki
    band = (dist >= 0) & (dist <= span)
    blk = jnp.arange(n_blk)[:, None, None]
    valid = band[None] & ((blk > 0) | (ki[None] >= span))
    scores = jnp.where(valid[None, :, :, None, None, :], scores, -jnp.inf)
    m = jnp.max(scores, axis=-1, keepdims=True)
    e = jnp.exp(scores - m)
    den = jnp.sum(e, axis=-1)
    num = jnp.einsum('bnqphk,bnkphd->bnqphd', e, vv.astype(jnp.float32))
    out = num / den[..., None]
    lse = m[..., 0] + jnp.log(den)
    out = out.reshape(b, n_blk * span, dilation, h, dh)[:, :n_sub].reshape(b, s, h, dh)
    lse = lse.reshape(b, n_blk * span, dilation, h)[:, :n_sub].reshape(b, s, h)
    return out, lse


def dilated_attention(q, k, v):
    outs, lses = [], []
    for g, (window, dilation) in enumerate(DILATED_GROUPS):
        sl = slice(g * N_SLOTS, (g + 1) * N_SLOTS)
        o, l = dilated_window_group(q[:, :, sl], k[:, :, sl], v[:, :, sl], window, dilation)
        outs.append(o)
        lses.append(l)
    out = jnp.stack(outs, axis=0)
    wts = jax.nn.softmax(jnp.stack(lses, axis=0), axis=0)
    return jnp.sum(wts[..., None] * out, axis=0)


def causal_depthwise_conv(x, w, b):
    c = x.shape[-1]
    y = lax.conv_general_dilated(x, w[:, None, :].astype(x.dtype), window_strides=(1,),
                                 padding=[(CONV_WIDTH - 1, 0)],
                                 dimension_numbers=('NWC', 'WIO', 'NWC'),
                                 feature_group_count=c)
    return y + b


def block_diag(x, w, b):
    xb = x.reshape(*x.shape[:-1], LRU_BLOCKS, LRU_BLOCK_DIM)
    y = jnp.einsum('bsni,nio->bsno', xb, w.astype(x.dtype))
    return y.reshape(x.shape) + b.astype(x.dtype)


def rg_lru(x, wx, bx, wa, ba, lam):
    xf = x.astype(jnp.float32)
    gate_i = jax.nn.sigmoid(block_diag(xf, wx, bx))
    gate_r = jax.nn.sigmoid(block_diag(xf, wa, ba))
    log_a = -LRU_C * gate_r * jax.nn.softplus(-lam.astype(jnp.float32))
    a = jnp.exp(log_a)
    mult = jnp.sqrt(-jnp.expm1(2.0 * log_a))
    b_in = mult * gate_i * xf

    def combine(left, right):
        a1, b1 = left
        a2, b2 = right
        return a1 * a2, a2 * b1 + b2

    _, h = lax.associative_scan(combine, (a, b_in), axis=1)
    return h.astype(x.dtype)


def expert_ffn_blocks(t, expert_id, weights, w1, w3, w2):
    n_tok, d = t.shape
    flat_e = expert_id.reshape(-1)
    flat_w = weights.reshape(-1)
    n_assign = flat_e.shape[0]
    order = jnp.argsort(flat_e)
    e_sorted = flat_e[order]
    tok_sorted = order // TOP_K_INNER
    sizes = jnp.bincount(flat_e, length=N_EXPERTS)
    starts = jnp.cumsum(sizes) - sizes
    padded = (sizes + MOE_BLOCK - 1) // MOE_BLOCK * MOE_BLOCK
    pad_ends = jnp.cumsum(padded)
    pad_starts = pad_ends - padded
    dest = pad_starts[e_sorted] + jnp.arange(n_assign) - starts[e_sorted]
    n_blocks = (n_assign + N_EXPERTS * (MOE_BLOCK - 1) + MOE_BLOCK - 1) // MOE_BLOCK
    xp = jnp.zeros((n_blocks * MOE_BLOCK, d), t.dtype).at[dest].set(t[tok_sorted])
    blk_e = jnp.minimum(jnp.searchsorted(pad_ends, jnp.arange(n_blocks) * MOE_BLOCK,
                                         side='right'), N_EXPERTS - 1)

    def run(args):
        xb, e = args
        hb = jax.nn.silu(xb @ w1[e]) * (xb @ w3[e])
        return hb @ w2[e]

    yp = lax.map(run, (xp.reshape(n_blocks, MOE_BLOCK, d), blk_e)).reshape(-1, d)
    ys = yp[dest] * flat_w[order][:, None].astype(yp.dtype)
    return jnp.zeros((n_tok, d), yp.dtype).at[tok_sorted].add(ys)


def hierarchical_moe(h, w_grp, b_grp, w_exp, b_exp, w1, w3, w2):
    b, s, d = h.shape
    t = h.reshape(b * s, d)
    grp_logits = (t @ w_grp + b_grp).astype(jnp.float32)
    grp_prob = jax.nn.softmax(grp_logits, axis=-1)
    grp_idx = jnp.argmax(grp_logits, axis=-1)
    grp_gate = jnp.take_along_axis(grp_prob, grp_idx[:, None], axis=-1)
    exp_logits = (t @ w_exp + b_exp).astype(jnp.float32).reshape(-1, N_EXPERT_GROUPS, EXPERTS_PER_GROUP)
    in_grp = jnp.take_along_axis(exp_logits, grp_idx[:, None, None], axis=1)[:, 0]
    top_val, top_idx = lax.top_k(in_grp, TOP_K_INNER)
    weights = grp_gate * jax.nn.softmax(top_val, axis=-1)
    expert_id = grp_idx[:, None].astype(jnp.int32) * EXPERTS_PER_GROUP + top_idx.astype(jnp.int32)
    out = expert_ffn_blocks(t, expert_id, weights, w1, w3, w2)
    return out.reshape(b, s, d)


def setup_inputs(seed: int = 0) -> dict:
    key = jax.random.key(seed)
    ks = jax.random.split(key, 25)
    f32 = jnp.float32
    L, D = DEPTH, D_MODEL

    def nrm(k, shape, scale):
        return jax.random.normal(k, shape, f32) * scale

    a0 = jax.random.uniform(ks[12], (L, LRU_WIDTH), f32, 0.9, 0.999)
    return {
        "x": nrm(ks[0], (BATCH, SEQ, D), 1.0),
        "c": nrm(ks[1], (BATCH, D), 1.0),
        "w_mod": nrm(ks[2], (L, D, 6 * D), 0.1 * D ** -0.5),
        "b_mod": nrm(ks[3], (L, 6 * D), 0.01),
        "norm1_g": 1.0 + nrm(ks[4], (L, D), 0.02),
        "w_in": nrm(ks[5], (L, D, IN_COLS), D ** -0.5),
        "conv_w": nrm(ks[6], (L, CONV_WIDTH, LRU_WIDTH), CONV_WIDTH ** -0.5),
        "conv_b": nrm(ks[7], (L, LRU_WIDTH), 0.01),
        "lru_wx": nrm(ks[8], (L, LRU_BLOCKS, LRU_BLOCK_DIM, LRU_BLOCK_DIM), LRU_BLOCK_DIM ** -0.5),
        "lru_bx": nrm(ks[9], (L, LRU_WIDTH), 0.01),
        "lru_wa": nrm(ks[10], (L, LRU_BLOCKS, LRU_BLOCK_DIM, LRU_BLOCK_DIM), LRU_BLOCK_DIM ** -0.5),
        "lru_ba": nrm(ks[11], (L, LRU_WIDTH), 0.01),
        "lru_lambda": jnp.log(a0) - jnp.log1p(-a0),
        "w_attn_o": nrm(ks[13], (L, ATTN_OUT_WIDTH, D), ATTN_OUT_WIDTH ** -0.5),
        "w_lru_o": nrm(ks[14], (L, LRU_WIDTH, D), LRU_WIDTH ** -0.5),
        "w_out": nrm(ks[15], (L, D, D), D ** -0.5),
        "norm2_g": 1.0 + nrm(ks[16], (L, D), 0.02),
        "w_grp": nrm(ks[17], (L, D, N_EXPERT_GROUPS), D ** -0.5),
        "b_grp": nrm(ks[18], (L, N_EXPERT_GROUPS), 0.01),
        "w_exp": nrm(ks[19], (L, D, N_EXPERTS), D ** -0.5),
        "b_exp": nrm(ks[20], (L, N_EXPERTS), 0.01),
        "w1": nrm(ks[21], (L, N_EXPERTS, D, D_EXPERT), D ** -0.5),
        "w3": nrm(ks[22], (L, N_EXPERTS, D, D_EXPERT), D ** -0.5),
        "w2": nrm(ks[23], (L, N_EXPERTS, D_EXPERT, D), D_EXPERT ** -0.5),
        "norm_f_g": 1.0 + nrm(ks[24], (D,), 0.02),
    }


def reference(x, c, w_mod, b_mod, norm1_g, w_in, conv_w, conv_b, lru_wx, lru_bx, lru_wa,
              lru_ba, lru_lambda, w_attn_o, w_lru_o, w_out, norm2_g, w_grp, b_grp, w_exp,
              b_exp, w1, w3, w2, norm_f_g):
    b, s, _ = x.shape
    c_act = jax.nn.silu(c)
    for l in range(DEPTH):
        mod = c_act @ w_mod[l] + b_mod[l]
        shift1, scale1, gate1, shift2, scale2, gate2 = jnp.split(mod[:, None, :], 6, axis=-1)

        h = rms_norm(x, norm1_g[l]) * (1.0 + scale1) + shift1
        proj = h @ w_in[l]
        q, k, v, xr, yr, gl = jnp.split(proj, IN_SPLITS, axis=-1)
        q = q.reshape(b, s, N_ATTN_HEADS, HEAD_DIM)
        k = k.reshape(b, s, N_ATTN_HEADS, HEAD_DIM)
        v = v.reshape(b, s, N_ATTN_HEADS, HEAD_DIM)
        attn = dilated_attention(q, k, v).reshape(b, s, ATTN_OUT_WIDTH).astype(x.dtype)

        xr = causal_depthwise_conv(xr, conv_w[l], conv_b[l])
        lru = rg_lru(xr, lru_wx[l], lru_bx[l], lru_wa[l], lru_ba[l], lru_lambda[l]) * jax.nn.gelu(yr)

        gates = jax.nn.sigmoid(gl.astype(jnp.float32)).astype(x.dtype)
        g_attn, g_lru = jnp.split(gates, N_BRANCHES, axis=-1)
        mixed = g_attn * (attn @ w_attn_o[l]) + g_lru * (lru @ w_lru_o[l])
        x = x + (1.0 + gate1) * (mixed @ w_out[l])

        h = rms_norm(x, norm2_g[l]) * (1.0 + scale2) + shift2
        x = x + (1.0 + gate2) * hierarchical_moe(h, w_grp[l], b_grp[l], w_exp[l], b_exp[l],
                                                 w1[l], w3[l], w2[l])
    return rms_norm(x, norm_f_g)
```

```python
import numpy as np
import concourse.bass as bass
import concourse.mybir as mybir
from concourse.bass_utils import run_bass_kernel_spmd

F32 = mybir.dt.float32
BF16 = mybir.dt.bfloat16
U32 = mybir.dt.uint32
I32 = mybir.dt.int32
U8 = mybir.dt.uint8
AF = mybir.ActivationFunctionType
ALU = mybir.AluOpType
AX = mybir.AxisListType

SEM_LIM = 8000
N_DMA_SLOTS = 16
PRIO_CRITICAL_PATH = True
PCP_W = 0.5
STRICT_SAME_ENGINE = True


class Buf:
    __slots__ = ("name", "w", "r")

    def __init__(self, name=""):
        self.name = name
        self.w = None
        self.r = []


class _Op:
    __slots__ = ("idx", "eng", "fn", "is_dma", "key", "nslots", "preds", "nsucc", "succs", "cost", "xfer",
                 "epoch", "pos", "ev", "ready", "npred", "finish", "rawsame", "tag", "prio", "tbl")

    def __init__(self):
        self.preds = set()
        self.succs = []
        self.rawsame = set()


class Sched:
    ENGS = ("pe", "dve", "act", "pool", "sp")

    def __init__(self, nc):
        self.nc = nc
        self.ops = []
        self.epoch = 0
        self.sems = []
        self.slots = {}
        self.final = None
        self.tag = "m"

    def _new_sem(self):
        h = self.nc.alloc_semaphore()
        self.sems.append(h)
        return len(self.sems) - 1

    def barrier(self):
        self.epoch += 1

    def _record(self, eng, fn, reads, writes, is_dma, key, nslots, cost, xfer, tbl=None):
        o = _Op()
        o.tbl = tbl
        o.idx = len(self.ops)
        o.eng = eng; o.fn = fn; o.is_dma = is_dma; o.key = key; o.nslots = nslots
        o.cost = cost; o.xfer = xfer; o.epoch = self.epoch; o.tag = self.tag
        for b in reads:
            if b.w is not None:
                o.preds.add(b.w)
                o.rawsame.add(b.w)
        for b in writes:
            if b.w is not None:
                o.preds.add(b.w)
            for r in b.r:
                o.preds.add(r)
        o.preds.discard(o.idx)
        self.ops.append(o)
        for b in writes:
            b.w = o.idx
            b.r = []
        for b in reads:
            b.r.append(o.idx)
        return o.idx

    def op(self, eng, fn, reads=(), writes=(), cost=0.3, tbl=None):
        return self._record(eng, fn, reads, writes, False, None, 0, cost, 0.0, tbl)

    def dma(self, eng, fn, reads=(), writes=(), pool=None, nslots=N_DMA_SLOTS, cost=None, xfer=4.0):
        key = eng if pool is None else eng + ":" + pool
        if cost is None:
            cost = 0.15 if eng == "sp" else 1.1
        return self._record(eng, fn, reads, writes, True, key, nslots, cost, xfer)

    def _schedule(self):
        import heapq
        ops = self.ops
        n = len(ops)
        ep_count = {}
        for o in ops:
            ep_count[o.epoch] = ep_count.get(o.epoch, 0) + 1
        for o in ops:
            o.npred = 0
            o.ready = 0.0
        for o in ops:
            keep = set()
            for p in o.preds:
                if ops[p].epoch == o.epoch:
                    keep.add(p)
            o.preds = keep
            o.npred = len(keep)
            for p in keep:
                ops[p].succs.append(o.idx)
        order = {e: [] for e in self.ENGS}
        free = {e: 0.0 for e in self.ENGS}
        cur_tbl = [None]
        skipped = [0]
        act_pick = [False]
        by_epoch = {}
        for o in ops:
            by_epoch.setdefault(o.epoch, []).append(o)
        tbase = 0.0
        for ep in sorted(by_epoch):
            eops = by_epoch[ep]
            for e in self.ENGS:
                free[e] = max(free[e], tbase)
            future = {e: [] for e in self.ENGS}
            now = {e: [] for e in self.ENGS}
            groups = {}
            for o in eops:
                groups.setdefault(o.tag, []).append(o)
            for g_ in groups.values():
                m_ = float(len(g_))
                for r_, o in enumerate(g_):
                    o.prio = r_ / m_
            if PRIO_CRITICAL_PATH:
                bl = {}
                for o in reversed(eops):
                    b_ = 0.0
                    for s_ in o.succs:
                        v_ = bl.get(s_, 0.0)
                        if v_ > b_:
                            b_ = v_
                    bl[o.idx] = b_ + o.cost + o.xfer
                mx_ = max(bl.values()) if bl else 1.0
                for o in eops:
                    o.prio = (1.0 - PCP_W) * o.prio + PCP_W * (1.0 - bl[o.idx] / mx_)
            for o in eops:
                if o.npred == 0:
                    o.ready = tbase
                    heapq.heappush(future[o.eng], (o.ready, o.prio, o.idx))
            left = len(eops)
            tmax = tbase
            while left:
                best = None
                for e in self.ENGS:
                    f = future[e]; nw = now[e]
                    while f and f[0][0] <= free[e]:
                        x_ = heapq.heappop(f)
                        heapq.heappush(nw, (x_[1], x_[2]))
                    if nw:
                        if e == "act" and len(nw) > 1:
                            head = nw[0]
                            ht = ops[head[1]].tbl
                            if ht is not None and ht != cur_tbl[0] and skipped[0] < 8:
                                pulled = [heapq.heappop(nw) for _ in range(min(10, len(nw)))]
                                pick = None
                                for it_ in pulled:
                                    t_ = ops[it_[1]].tbl
                                    if t_ is None or t_ == cur_tbl[0]:
                                        pick = it_
                                        break
                                for it_ in pulled:
                                    if it_ is not pick:
                                        heapq.heappush(nw, it_)
                                if pick is not None:
                                    heapq.heappush(nw, (-1.0, pick[1]))
                                    act_pick[0] = True
                        cand = (free[e], nw[0][0], nw[0][1], e, True)
                    elif f:
                        cand = (f[0][0], f[0][1], f[0][2], e, False)
                    else:
                        continue
                    if best is None or cand[:3] < best[:3]:
                        best = cand
                start, _pr, idx, e, from_now = best
                if from_now:
                    heapq.heappop(now[e])
                else:
                    heapq.heappop(future[e])
                o = ops[idx]
                if e == "act":
                    if act_pick[0] and from_now:
                        skipped[0] += 1
                    else:
                        skipped[0] = 0
                    if o.tbl is not None and o.tbl != cur_tbl[0]:
                        o.cost += 1.3
                        cur_tbl[0] = o.tbl
                act_pick[0] = False
                o.pos = len(order[e])
                order[e].append(o)
                free[e] = start + o.cost
                o.finish = start + o.cost + o.xfer
                tmax = max(tmax, o.finish)
                left -= 1
                for s in o.succs:
                    so = ops[s]
                    so.npred -= 1
                    if so.ready < o.finish:
                        so.ready = o.finish
                    if so.npred == 0:
                        heapq.heappush(future[so.eng], (so.ready, so.prio, so.idx))
            tbase = tmax
        self.est_us = tbase
        return order

    def replay(self):
        nc = self.nc
        ops = self.ops
        order = self._schedule()
        chains = {e: [] for e in self.ENGS}
        cnt = {e: 0 for e in self.ENGS}
        slots = {}
        rr = {}
        slot_wait = {}
        for e in self.ENGS:
            for o in order[e]:
                if o.is_dma:
                    sl = slots.setdefault(o.key, [])
                    if len(sl) < o.nslots:
                        sl.append([self._new_sem(), 0])
                        slot = sl[-1]
                    else:
                        k = rr.get(o.key, 0)
                        rr[o.key] = k + 1
                        slot = sl[k % o.nslots]
                        if (slot[1] + 1) * 16 > SEM_LIM * 4:
                            slot[0] = self._new_sem()
                            slot[1] = 0
                    if slot[1] > 0:
                        slot_wait[o.idx] = (slot[0], slot[1] * 16)
                    slot[1] += 1
                    o.ev = (slot[0], slot[1] * 16, 16)
                else:
                    j, v = divmod(cnt[e], SEM_LIM)
                    if j >= len(chains[e]):
                        chains[e].append(self._new_sem())
                    cnt[e] += 1
                    o.ev = (chains[e][j], v + 1, 1)
        self.slots = slots
        nep = self.epoch + 1
        ep_last = [dict() for _ in range(nep)]
        for o in ops:
            d = ep_last[o.epoch]
            s, v, _ = o.ev
            if d.get(s, 0) < v:
                d[s] = v
        cum = []
        acc = {}
        for k in range(nep):
            cum.append(dict(acc))
            for s, v in ep_last[k].items():
                if acc.get(s, 0) < v:
                    acc[s] = v
        self.final = acc
        sems = self.sems
        prog = {}
        for e in self.ENGS:
            own = set(chains[e])
            seen = {}
            lst = []
            cur_ep = -1
            for o in order[e]:
                deps = {}
                if o.epoch != cur_ep:
                    cur_ep = o.epoch
                    for s, v in cum[o.epoch].items():
                        if s not in own:
                            deps[s] = v
                for p in o.preds:
                    po = ops[p]
                    s, v, _ = po.ev
                    if s in own:
                        if e == "pe" or (p not in o.rawsame and not STRICT_SAME_ENGINE):
                            continue
                    if deps.get(s, 0) < v:
                        deps[s] = v
                if o.idx in slot_wait:
                    s, v = slot_wait[o.idx]
                    if deps.get(s, 0) < v:
                        deps[s] = v
                waits = []
                for s, v in deps.items():
                    if seen.get(s, 0) < v:
                        seen[s] = v
                        waits.append((s, v))
                lst.append((waits, o.fn, o.ev))
            prog[e] = (lst, own)
        final_waits = [(s, v) for s, v in self.final.items()]
        with nc.Block() as block:
            for e in self.ENGS:
                lst, own = prog[e]
                starter = {"pe": block.tensor, "dve": block.vector, "act": block.scalar,
                           "pool": block.gpsimd, "sp": block.sync}[e]

                def body(engine, lst=lst, e=e, own=own):
                    for waits, fn, (s, v, inc) in lst:
                        for ws, wv in waits:
                            engine.wait_ge(sems[ws], wv)
                        ins = fn()
                        ins.then_inc(sems[s], inc)
                    if e == "sp":
                        for ws, wv in final_waits:
                            if ws not in own:
                                engine.wait_ge(sems[ws], wv)
                starter(body)


NCORES = 8
NS = 2
SEQ = 2048
D = 1024
NT = SEQ // 128
TT = NS * NT
IN_COLS = 8704
NBLK = 96
EPS = 1e-6


class Ctx:
    pass


def build_program(stage=99, dbg=()):
    nc = bass.Bass("TRN2", target_bir_lowering=False)
    S = Sched(nc)
    K = Ctx()

    def din(name, shape, dt=F32):
        return nc.dram_tensor(name, list(shape), dt, kind="ExternalInput").ap()

    def dscr(name, shape, dt):
        return nc.dram_tensor(name, list(shape), dt, kind="Internal").ap()

    x = din("x", [NS, SEQ, D])
    cT = din("cT", [128, 8, NS])
    w_mod = din("w_mod", [D, 6 * D])
    b_modT = din("b_modT", [128, 48])
    gT = din("gT", [128, 2, 8])
    gf_rep = din("gf_rep", [128, D])
    w_in = din("w_in", [D, IN_COLS])
    conv_wT = din("conv_wT", [128, 8, 4])
    lru_vecs = din("lru_vecs", [128, 4, 8])
    wx_blk = din("wx_blk", [8, 128, 128])
    wa_blk = din("wa_blk", [8, 128, 128])
    w_attn_o = din("w_attn_o", [512, D])
    w_lru_o = din("w_lru_o", [D, D])
    w_out = din("w_out", [D, D])
    wr = din("wr", [128, 8, 36])
    br_rep = din("br_rep", [128, 36])
    w1 = din("w1", [4096, 4096])
    w3 = din("w3", [4096, 4096])
    w2 = din("w2", [4096, 4096])
    out = nc.dram_tensor("out", [NS, SEQ, D], F32, kind="ExternalOutput").ap()
    x1_d = dscr("x1_d", [TT * 128, D], F32)
    h2_d = dscr("h2_d", [TT * 128, D], BF16)
    xp_d = dscr("xp_d", [NBLK * 128, D], BF16)
    yp_d = dscr("yp_d", [NBLK * 128, D], F32)
    w1b = dscr("w1b", [4096, 4096], BF16)
    w3b = dscr("w3b", [4096, 4096], BF16)
    w2b = dscr("w2b", [4096, 4096], BF16)
    conv_jobs = []
    for e in range(32):
        for (dst_, src_) in ((w1b, w1), (w3b, w3), (w2b, w2)):
            conv_jobs.append((dst_[e * 128:(e + 1) * 128, :].rearrange("r (a n) -> r a n", a=2),
                              src_[e * 128:(e + 1) * 128, :].rearrange("r (a n) -> r a n", a=2)))

    def issue_conv(n, after=()):
        for _ in range(n):
            if not conv_jobs:
                return
            d_, s_ = conv_jobs.pop(0)
            S.dma("pool", lambda d_=d_, s_=s_: nc.gpsimd.dma_start(out=d_, in_=s_), list(after), [], pool="conv", nslots=16, xfer=20.0)
    dbg_out = {}

    TOT = 206 * 1024
    arena = nc.alloc_sbuf_tensor("arena", [128, TOT], U8).ap()
    st = {"off": 0}
    SZ = {F32: 4, BF16: 2, U32: 4, I32: 4}

    def carve(shape, dt):
        n = int(np.prod(shape[1:])) * SZ[dt]
        assert st["off"] + n <= TOT, ("SBUF overflow", st["off"], n)
        t = arena[:, st["off"]:st["off"] + n].bitcast(dt)
        st["off"] += (n + 63) // 64 * 64
        if len(shape) == 3:
            t = t.rearrange("p (a b) -> p a b", a=shape[1])
        elif len(shape) == 4:
            t = t.rearrange("p (a b c) -> p a b c", a=shape[1], b=shape[2])
        return t

    def mark():
        return st["off"]

    def release(m):
        S.barrier()
        st["off"] = m

    banks = [nc.alloc_psum_tensor("bank%d" % i, [128, 512], F32).ap() for i in range(8)]
    bank_b = [Buf("bank%d" % i) for i in range(8)]
    bk = {"i": 0, "o": 0, "c": 0}

    def bank():
        if S.tag == "B":
            i = 2 + bk["i"] % 4
            bk["i"] += 1
        elif S.tag == "C":
            i = 6 + bk["c"] % 2
            bk["c"] += 1
        else:
            i = 2 + bk["i"] % 6
            bk["i"] += 1
        return banks[i], bank_b[i]

    def bank_o():
        i = bk["o"] % 2
        bk["o"] += 1
        return banks[i], bank_b[i]

    DSZ = {F32: 4, BF16: 2, U32: 4, I32: 4, U8: 1}

    def fsz(ap):
        n = 1
        for s_ in ap.shape[1:]:
            n *= int(s_)
        return n

    def act(out_, in_, func, R, W, bias=None, scale=None, accum=None):
        kw = {}
        if bias is not None:
            kw["bias"] = bias
        if scale is not None:
            kw["scale"] = scale
        if accum is not None:
            kw["accum_out"] = accum
        tbl = {AF.Exp: "e", AF.Ln: "e", AF.Sigmoid: "s", AF.Sqrt: "q"}.get(func)
        S.op("act", lambda: nc.scalar.activation(out=out_, in_=in_, func=func, **kw), R, W,
             cost=0.2 + fsz(out_) / 1000.0, tbl=tbl)

    def ecost(eng, n, slow=1.0):
        if eng == "pool":
            return 0.25 + n / 435.0
        return 0.12 + slow * n / 1250.0

    def tt(eng, out_, in0, in1, op, R, W):
        e = nc.vector if eng == "dve" else nc.gpsimd
        S.op(eng, lambda: e.tensor_tensor(out=out_, in0=in0, in1=in1, op=op), R, W, cost=ecost(eng, fsz(out_), 1.55))

    def ts(eng, out_, in0, s1, s2, op0, op1, R, W):
        e = nc.vector if eng == "dve" else nc.gpsimd
        c_ = ecost(eng, fsz(out_))
        if op1 is None:
            S.op(eng, lambda: e.tensor_scalar(out=out_, in0=in0, scalar1=s1, scalar2=None, op0=op0), R, W, cost=c_)
        else:
            S.op(eng, lambda: e.tensor_scalar(out=out_, in0=in0, scalar1=s1, scalar2=s2, op0=op0, op1=op1), R, W, cost=c_)

    def stt(out_, in0, scalar, in1, op0, op1, R, W):
        S.op("dve", lambda: nc.vector.scalar_tensor_tensor(out=out_, in0=in0, scalar=scalar, in1=in1,
                                                           op0=op0, op1=op1), R, W, cost=ecost("dve", fsz(out_), 1.55))

    def cp(eng, out_, in_, R, W):
        if eng == "act":
            S.op("act", lambda: nc.scalar.copy(out=out_, in_=in_), R, W, cost=0.2 + fsz(out_) / 1000.0)
        else:
            e = nc.vector if eng == "dve" else nc.gpsimd
            S.op(eng, lambda: e.tensor_copy(out=out_, in_=in_), R, W, cost=ecost(eng, fsz(out_)))

    def mm(out_, lhsT, rhs, start, stop, R, W):
        S.op("pe", lambda: nc.tensor.matmul(out_, lhsT=lhsT, rhs=rhs, start=start, stop=stop), R, W,
             cost=max(0.065, fsz(rhs) / 1400.0) * (4.0 if rhs.dtype == F32 else 1.0))

    def tr(out_, in_, ident, R, W):
        S.op("pe", lambda: nc.tensor.transpose(out=out_, in_=in_, identity=ident), R, W, cost=0.1)

    def dma(q, out_, in_, R, W):
        e = nc.sync if q == "sp" else nc.gpsimd
        nbytes = fsz(out_) * int(out_.shape[0]) * DSZ[out_.dtype]
        return S.dma(q, lambda: e.dma_start(out=out_, in_=in_), R, W, xfer=4.0 + nbytes / 100e3)

    def gather(out_, in_, idx, R, W):
        nbytes = fsz(out_) * int(out_.shape[0]) * DSZ[out_.dtype]
        return S.dma("pool", lambda: nc.gpsimd.indirect_dma_start(
            out=out_, out_offset=None, in_=in_,
            in_offset=bass.IndirectOffsetOnAxis(ap=idx, axis=0)), R, W, xfer=4.0 + nbytes / 100e3)

    def scatter(out_, idx, in_, R, W):
        nbytes = fsz(in_) * int(in_.shape[0]) * DSZ[in_.dtype]
        return S.dma("pool", lambda: nc.gpsimd.indirect_dma_start(
            out=out_, out_offset=bass.IndirectOffsetOnAxis(ap=idx, axis=0), in_=in_,
            in_offset=None), R, W, xfer=4.0 + nbytes / 100e3)

    def dump(name, sb_ap, shape, dt, R):
        if name not in dbg:
            return
        d = nc.dram_tensor("dbg_" + name, list(shape), dt, kind="ExternalOutput").ap()
        dbg_out[name] = d
        dma("sp", d, sb_ap, R, [])

    ident_f = carve([128, 128], F32); B_const = Buf("const")
    ident_b = carve([128, 128], BF16)
    iota_i = carve([128, 128], I32)
    mask = carve([128, 256], BF16)
    ustrict = carve([128, 128], BF16)
    ones_b = carve([128, 128], BF16)
    zeros_f = carve([128, 128], F32)
    zeros_b8 = zeros_f.bitcast(BF16).rearrange("p (a n) -> p a n", a=2)[:, 0:1, :].to_broadcast([128, 8, 128])
    cst = carve([128, 4], F32)
    modT = carve([128, 48, NS], F32); B_mod = Buf("mod")
    B_mod1 = Buf("mod1")
    B_modc = Buf("modc")
    gT_sb = carve([128, 2, 8], F32)
    bmod_sb = carve([128, 48], F32)
    der = carve([128, 6, 8, NS], F32)
    convw_sb = carve([128, 8, 4], F32)
    lruv_sb = carve([128, 4, 8], F32)
    cl_sb = carve([128, 3, 8], F32)
    wxb_sb = carve([128, 8, 128], BF16)
    wab_sb = carve([128, 8, 128], BF16)
    B_lruc = Buf("lruconst")
    wr_sb = carve([128, 8, 36], F32)
    br_sb = carve([128, 36], F32)
    B_wr = Buf()
    A0_all = carve([128, TT, 32], BF16)
    A1_all = carve([128, TT, 32], BF16)
    As_all = carve([128, TT, 32], BF16)
    gw_all = carve([128, 2, TT], F32)
    dest_all = carve([128, 2, TT], U32)
    B_rt = [Buf("rt%d" % t) for t in range(TT)]
    B_dest = [Buf("dest%d" % t) for t in range(TT)]

    S.op("pool", lambda: nc.gpsimd.iota(iota_i, pattern=[[1, 128]], base=0, channel_multiplier=-1), [], [B_const])
    ts("dve", ident_f, iota_i, 0, None, ALU.is_equal, None, [B_const], [B_const])
    ts("dve", ident_b, iota_i, 0, None, ALU.is_equal, None, [B_const], [B_const])
    ts("dve", mask[:, 0:128], iota_i, 0, None, ALU.is_ge, None, [B_const], [B_const])
    ts("dve", mask[:, 128:256], iota_i, 0, None, ALU.is_le, None, [B_const], [B_const])
    ts("dve", ustrict, iota_i, 0, None, ALU.is_gt, None, [B_const], [B_const])
    S.op("dve", lambda: nc.vector.memset(ones_b, 1.0), [], [B_const])
    S.op("dve", lambda: nc.vector.memset(zeros_f, 0.0), [], [B_const])
    S.op("dve", lambda: nc.vector.memset(cst[:, 0:1], EPS), [], [B_const])
    S.op("dve", lambda: nc.vector.memset(cst[:, 1:2], 1.0), [], [B_const])
    ts("dve", cst[:, 2:3], iota_i[:, 0:1], -1.0, None, ALU.mult, None, [B_const], [B_const])
    eps_ap = cst[:, 0:1]
    one_ap = cst[:, 1:2]

    dma("sp", gT_sb, gT, [], [B_modc])
    dma("sp", bmod_sb, b_modT, [], [B_modc])
    dma("sp", convw_sb, conv_wT, [], [B_lruc])
    dma("sp", lruv_sb, lru_vecs, [], [B_lruc])
    dma("sp", wr_sb, wr, [], [B_wr])
    dma("sp", br_sb, br_rep, [], [B_wr])
    dma("pool", wxb_sb, wx_blk.rearrange("c i o -> i c o"), [], [B_lruc])
    dma("pool", wab_sb, wa_blk.rearrange("c i o -> i c o"), [], [B_lruc])
    act(cl_sb[:, 0, :], lruv_sb[:, 3, :], AF.Exp, [B_lruc], [B_lruc], scale=-1.0)
    act(cl_sb[:, 0, :], cl_sb[:, 0, :], AF.Ln, [B_lruc, B_const], [B_lruc], bias=one_ap)
    ts("dve", cl_sb[:, 1, :], cl_sb[:, 0, :], -8.0, None, ALU.mult, None, [B_lruc], [B_lruc])
    ts("dve", cl_sb[:, 2, :], cl_sb[:, 0, :], -16.0, None, ALU.mult, None, [B_lruc], [B_lruc])

    m0 = mark()
    st["off"] = 176 * 1024
    S.tag = "C"
    cact = carve([128, 8, NS], F32); B_cact = Buf()
    csig = carve([128, 8, NS], F32)
    dma("sp", cact, cT, [], [B_cact])
    act(csig, cact, AF.Sigmoid, [B_cact], [B_cact])
    tt("dve", cact, cact, csig, ALU.mult, [B_cact], [B_cact])
    cact_b = carve([128, 8, NS], BF16)
    cp("dve", cact_b, cact, [B_cact], [B_cact])
    wm = [carve([128, 8, 512], BF16) for _ in range(3)]
    B_wm = [Buf(), Buf(), Buf()]
    for blk in range(12):
        w_ = wm[blk % 3]; bw = B_wm[blk % 3]
        dma("pool", w_, w_mod[:, blk * 512:(blk + 1) * 512].rearrange("(c p) n -> p c n", p=128), [], [bw])
        ps, pb = bank()
        for o4 in range(4):
            for kc in range(8):
                mm(ps[:, o4 * 2:o4 * 2 + 2], w_[:, kc, o4 * 128:(o4 + 1) * 128], cact_b[:, kc, :],
                   kc == 0, kc == 7, [bw, B_cact], [pb])
        for o4 in range(4):
            oc = blk * 4 + o4
            ts("dve", modT[:, oc, :], ps[:, o4 * 2:o4 * 2 + 2], bmod_sb[:, oc:oc + 1], None, ALU.add, None,
               [pb, B_modc], [B_mod1 if oc < 16 else B_mod])
    for b in range(NS):
        stt(der[:, 0, :, b], modT[:, 8:16, b], 1.0, gT_sb[:, 0, :], ALU.add, ALU.mult, [B_mod1, B_modc], [B_mod1])
        stt(der[:, 1, :, b], modT[:, 32:40, b], 1.0, gT_sb[:, 1, :], ALU.add, ALU.mult, [B_mod, B_modc], [B_mod])
        ts("dve", der[:, 2, :, b], modT[:, 16:24, b], 1.0, None, ALU.add, None, [B_mod], [B_mod])
        ts("dve", der[:, 3, :, b], modT[:, 40:48, b], 1.0, None, ALU.add, None, [B_mod], [B_mod])
    dump("modT", modT, [128, 48, NS], F32, [B_mod, B_mod1])
    st["off"] = m0
    S.tag = "m"

    bc_t = [carve([128, 128], F32) for _ in range(2)]
    B_bc = [Buf(), Buf()]
    rr = {"i": 0}

    def replicate(dst, dst_b, vec_ap_fn):
        for half in range(2):
            ps, pb = bank()
            for c4 in range(4):
                c = half * 4 + c4
                i = rr["i"] % 2; rr["i"] += 1
                act(bc_t[i], zeros_f, AF.Identity, [B_const, B_mod], [B_bc[i]], bias=vec_ap_fn(c))
                mm(ps[:, c4 * 128:(c4 + 1) * 128], bc_t[i], ident_f, True, True, [B_bc[i], B_const], [pb])
            cp("dve", dst[:, half * 512:(half + 1) * 512], ps, [pb], [dst_b])

    def tokview(X, g):
        if g == 0:
            return X.rearrange("d (t j) -> d t j", j=128)
        if g == 1:
            return X.rearrange("d (n j r) -> d r n j", n=4, j=128, r=4)
        return X.rearrange("d (j r) -> d r j", r=16)

    def tile_ap(X, g, t):
        v = tokview(X, g)
        if g == 1:
            return v[:, t // 4, t % 4, :]
        return v[:, t, :]

    def pair_ap(X, g, t):
        v = tokview(X, g)
        if g == 0:
            return v[:, t:t + 2, :]
        return v[:, t // 4, (t % 4):(t % 4) + 2, :]

    def unit_ap(X, g, u):
        v = tokview(X, g)
        if g == 1:
            return v[:, u, :, :]
        return v[:, 4 * u:4 * u + 4, :]

    def has_next(g, t):
        return (g == 0 and t < 15) or (g == 1 and t % 4 < 3)

    def has_prev(g, t):
        return (g == 0 and t > 0) or (g == 1 and t % 4 > 0)

    def wcols(col0, n=128):
        return w_in[:, col0:col0 + n].rearrange("(c p) n -> p c n", p=128)

    def rms_stats(src, bsrc, junk, bjunk, sq, bsq):
        act(junk, src, AF.Square, [bsrc], [bjunk, bsq], accum=sq[:, 0:1])
        act(sq[:, 1:2], sq[:, 0:1], AF.Ln, [bsq, B_const], [bsq], bias=eps_ap, scale=1.0 / D)
        act(sq[:, 2:3], sq[:, 1:2], AF.Exp, [bsq], [bsq], scale=-0.5)

    def phase_seq(b):
        mseq = mark()
        hT = carve([128, 8, SEQ], BF16); B_hT = [Buf("hT%d" % i) for i in range(4)]
        attnT = carve([128, 4, SEQ], BF16); B_attn = [Buf("attn%d" % i) for i in range(4)]
        lruT = carve([128, 8, SEQ], BF16); B_lru = [Buf("lru%d" % i) for i in range(8)]

        mA = mark()
        S.tag = "B"
        xt = [carve([128, D], F32) for _ in range(2)]; B_xt = [Buf(), Buf()]
        xn = [carve([128, 4, D], F32) for _ in range(2)]; B_xn = [Buf(), Buf()]
        junk = carve([128, D], BF16); B_junk = Buf()
        sq = [carve([128, 4], F32) for _ in range(4)]; B_sq = [Buf() for _ in range(4)]
        k = 0
        for grp in range(4):
            xg = xn[grp % 2]; bxg = B_xn[grp % 2]
            for i4 in range(4):
                ti = grp * 4 + i4
                xti = xt[k % 2]; bxt = B_xt[k % 2]
                sqk = sq[k % 4]; bsq = B_sq[k % 4]
                k += 1
                dma("sp", xti, x[b, ti * 128:(ti + 1) * 128, :], [], [bxt])
                rms_stats(xti, bxt, junk, B_junk, sqk, bsq)
                ts("dve", xg[:, i4, :], xti, sqk[:, 2:3], None, ALU.mult, None, [bxt, bsq], [bxg])
            for c in range(8):
                ps, pb = bank()
                for i4 in range(4):
                    tr(ps[:, i4 * 128:(i4 + 1) * 128], xg[:, i4, c * 128:(c + 1) * 128], ident_f, [bxg, B_const], [pb])
                act(hT[:, c, grp * 512:(grp + 1) * 512], ps, AF.Identity, [pb, B_mod1], [B_hT[grp]],
                    bias=modT[:, c, b:b + 1], scale=der[:, 0, c, b:b + 1])
        if b == 0:
            dump("hT", hT, [128, 8, SEQ], BF16, B_hT)
        S.tag = "m"
        release(mA)
        if stage < 2:
            release(mseq)
            return

        mB = mark()
        S.tag = "B"
        wq = [carve([128, 8, 128], BF16) for _ in range(2)]; B_wq = [Buf(), Buf()]
        wk = [carve([128, 8, 128], BF16) for _ in range(2)]; B_wk = [Buf(), Buf()]
        wv = [carve([128, 8, 128], BF16) for _ in range(2)]; B_wv = [Buf(), Buf()]
        qT2_ = carve([128, SEQ], BF16); qT2 = [qT2_, qT2_]; B_q_ = Buf(); B_q = [B_q_, B_q_]
        kT2_ = carve([128, SEQ], BF16); kT2 = [kT2_, kT2_]; B_k_ = Buf(); B_k = [B_k_, B_k_]
        V_sb = [carve([128, 16, 256], BF16) for _ in range(2)]; B_V = [Buf(), Buf()]
        NP = 6
        Pexp = [carve([128, 256], BF16) for _ in range(NP)]; B_pe = [Buf() for _ in range(NP)]
        Pm = [carve([128, 256], BF16) for _ in range(NP)]; B_pm = [Buf() for _ in range(NP)]
        acc = [carve([128, SEQ], F32) for _ in range(2)]; B_acc = [Buf(), Buf()]
        rtmp = carve([128, SEQ], F32); B_rtmp = Buf()
        for i in range(2):
            S.op("pool", lambda v=V_sb[i]: nc.gpsimd.memset(v[:, :, 64:192], 1.0), [], [B_V[i]])
        it = 0
        pk = [0]
        for sp in range(4):
            for g in range(3):
                col0 = g * 512 + sp * 128
                i = it % 2; it += 1
                dma("pool", wq[i], wcols(col0), [], [B_wq[i]])
                dma("pool", wk[i], wcols(1536 + col0), [], [B_wk[i]])
                dma("pool", wv[i], wcols(3072 + col0), [], [B_wv[i]])
                for (w_, bw, dst, bdst, sc) in ((wq[i], B_wq[i], qT2[i], B_q[i], 0.125),
                                                (wk[i], B_wk[i], kT2[i], B_k[i], 1.0)):
                    for tb in range(4):
                        ps, pb = bank()
                        for kc in range(8):
                            mm(ps, w_[:, kc, :], hT[:, kc, tb * 512:(tb + 1) * 512], kc == 0, kc == 7,
                               [bw, B_hT[tb]], [pb])
                        ts("dve", dst[:, tb * 512:(tb + 1) * 512], ps, sc, None, ALU.mult, None, [pb], [bdst])
                for u in range(4):
                    ps, pb = bank()
                    for t4 in range(4):
                        t = u * 4 + t4
                        for kc in range(8):
                            mm(ps[:, t4 * 128:(t4 + 1) * 128], tile_ap(hT[:, kc, :], g, t), wv[i][:, kc, :],
                               kc == 0, kc == 7, [B_wv[i]] + B_hT, [pb])
                    psv = ps.rearrange("p (a b) -> p a b", a=4)
                    cp("act", V_sb[i][:, u * 4:(u + 1) * 4, 0:64], psv[:, :, 0:64], [pb], [B_V[i]])
                    cp("dve", V_sb[i][:, u * 4:(u + 1) * 4, 192:256], psv[:, :, 64:128], [pb], [B_V[i]])

                items = [(hd, t) for hd in range(2) for t in range(16)]
                slot_of = {}
                obs = {}

                def stage1(hd, t, i=i, g=g):
                    rows = slice(hd * 64, hd * 64 + 64)
                    qh = qT2[i][rows, :]; kh = kT2[i][rows, :]
                    ps, pb = bank()
                    nx = has_next(g, t)
                    N = 256 if nx else 128
                    if nx:
                        mm(ps[:, 0:256].rearrange("p (a b) -> p a b", a=2), tile_ap(kh, g, t), pair_ap(qh, g, t),
                           True, True, [B_k[i], B_q[i]], [pb])
                    else:
                        mm(ps[:, 0:128], tile_ap(kh, g, t), tile_ap(qh, g, t), True, True, [B_k[i], B_q[i]], [pb])
                    j = pk[0] % NP; pk[0] += 1
                    slot_of[(hd, t)] = j
                    act(Pexp[j][:, 0:N], ps[:, 0:N], AF.Exp, [pb], [B_pe[j]])
                    tt("dve", Pm[j][:, 0:N], Pexp[j][:, 0:N], mask[:, 0:N], ALU.mult, [B_pe[j], B_const], [B_pm[j]])

                def stage2(hd, t, i=i, g=g):
                    u, t4 = divmod(t, 4)
                    if t4 == 0:
                        obs[hd] = bank_o()
                    ob, obb = obs[hd]
                    j = slot_of[(hd, t)]
                    lv = V_sb[i][:, t, hd * 128:(hd + 1) * 128]
                    oc_ = ob[:, t4 * 128:(t4 + 1) * 128]
                    if has_prev(g, t):
                        pj = slot_of[(hd, t - 1)]
                        mm(oc_, V_sb[i][:, t - 1, hd * 128:(hd + 1) * 128], Pm[pj][:, 128:256], True, False,
                           [B_V[i], B_pm[pj]], [obb])
                        mm(oc_, lv, Pm[j][:, 0:128], False, True, [B_V[i], B_pm[j]], [obb])
                    else:
                        mm(oc_, lv, Pm[j][:, 0:128], True, True, [B_V[i], B_pm[j]], [obb])
                    if t4 == 3:
                        dst = unit_ap(acc[hd], g, u)
                        obv = ob.rearrange("p (a b) -> p a b", a=4)
                        if g == 0:
                            cp("dve", dst, obv, [obb], [B_acc[hd]])
                        else:
                            tt("dve", dst, obv, dst, ALU.add, [obb, B_acc[hd]], [B_acc[hd]])

                LA = 2
                for idx in range(len(items) + LA):
                    if idx < len(items):
                        stage1(*items[idx])
                    if idx >= LA:
                        stage2(*items[idx - LA])
            act(rtmp[0:64, :], acc[0][64:128, :], AF.Ln, [B_acc[0]], [B_rtmp])
            act(rtmp[0:64, :], rtmp[0:64, :], AF.Exp, [B_rtmp], [B_rtmp], scale=-1.0)
            tt("dve", attnT[0:64, sp, :], acc[0][0:64, :], rtmp[0:64, :], ALU.mult, [B_acc[0], B_rtmp], [B_attn[sp]])
            act(rtmp[64:128, :], acc[1][0:64, :], AF.Ln, [B_acc[1]], [B_rtmp])
            act(rtmp[64:128, :], rtmp[64:128, :], AF.Exp, [B_rtmp], [B_rtmp], scale=-1.0)
            tt("dve", attnT[64:128, sp, :], acc[1][64:128, :], rtmp[64:128, :], ALU.mult, [B_acc[1], B_rtmp], [B_attn[sp]])
        if b == 0:
            dump("attnT", attnT, [128, 4, SEQ], BF16, B_attn)

        mC = mark()
        S.tag = "C"
        wxr = [carve([128, 8, 128], BF16) for _ in range(2)]; B_wxr = [Buf(), Buf()]
        wyr = [carve([128, 8, 128], BF16) for _ in range(2)]; B_wyr = [Buf(), Buf()]
        NQ = 4
        HL = SEQ // NQ
        RS = [[carve([128, HL + 16], F32)] + [carve([128, HL], F32) for _ in range(3)] for _ in range(2)]
        B_RS = [[Buf() for _ in range(4)] for _ in range(2)]
        YS = [[carve([128, HL], F32) for _ in range(2)] for _ in range(2)]
        B_YS = [[Buf(), Buf()] for _ in range(2)]
        xcbs = [carve([128, HL], BF16) for _ in range(2)]; B_xcbs = [Buf(), Buf()]
        halo = carve([128, 4], F32); B_halo = Buf()
        un = 0
        for c in range(8):
            i = c % 2
            dma("pool", wxr[i], wcols(4608 + c * 128), [], [B_wxr[i]])
            dma("pool", wyr[i], wcols(5632 + c * 128), [], [B_wyr[i]])
            for hf in range(NQ):
                s = un % 2; un += 1
                R1, R2, R3, R4 = RS[s]; b1, b2, b3, b4 = B_RS[s]
                Y1, Y2 = YS[s]; by1, by2 = B_YS[s]
                xcb = xcbs[s]; bxc = B_xcbs[s]
                pR2 = RS[1 - s][1]; pb2 = B_RS[1 - s][1]
                if hf == 0:
                    S.op("pool", lambda R1=R1: nc.gpsimd.memset(R1[:, 0:3], 0.0), [], [b1])
                else:
                    cp("pool", R1[:, 0:3], halo[:, 0:3], [B_halo], [b1])
                for t2 in range(1):
                    tb = hf
                    ps, pb = bank()
                    for kc in range(8):
                        mm(ps, wxr[i][:, kc, :], hT[:, kc, tb * 512:(tb + 1) * 512], kc == 0, kc == 7, [B_wxr[i], B_hT[tb]], [pb])
                    cp("act", R1[:, 3 + t2 * 512:3 + (t2 + 1) * 512], ps, [pb], [b1])
                if hf < NQ - 1:
                    cp("pool", halo[:, 0:3], R1[:, HL:HL + 3], [b1], [B_halo])
                ts("dve", R2, R1[:, 3:3 + HL], convw_sb[:, c, 3:4], lruv_sb[:, 0, c:c + 1], ALU.mult, ALU.add,
                   [b1, B_lruc], [b2])
                for j in range(3):
                    stt(R2, R1[:, j:j + HL], convw_sb[:, c, j:j + 1], R2, ALU.mult, ALU.add, [b1, b2, B_lruc], [b2])
                cp("act", xcb, R2, [b2], [bxc])
                issue_conv(2, after=[bxc])
                for (wb, bias_ap, dst, bdst) in ((wxb_sb, lruv_sb[:, 1, c:c + 1], R3, b3), (wab_sb, lruv_sb[:, 2, c:c + 1], R4, b4)):
                    for t2 in range(1):
                        ps, pb = bank()
                        mm(ps, wb[:, c, :], xcb[:, t2 * 512:(t2 + 1) * 512], True, True, [B_lruc, bxc], [pb])
                        act(dst[:, t2 * 512:(t2 + 1) * 512], ps, AF.Sigmoid, [pb, B_lruc], [bdst], bias=bias_ap)
                act(R1[:, 0:HL], R4, AF.Exp, [b4, B_lruc], [b1], scale=cl_sb[:, 1, c:c + 1])
                act(R4, R4, AF.Exp, [b4, B_lruc], [b4], scale=cl_sb[:, 2, c:c + 1])
                act(R4, R4, AF.Ln, [b4, B_const], [b4], bias=one_ap, scale=-1.0)
                act(R4, R4, AF.Exp, [b4], [b4], scale=0.5)
                tt("pool", R3, R3, R2, ALU.mult, [b3, b2], [b3])
                tt("pool", R3, R3, R4, ALU.mult, [b3, b4], [b3])
                if hf == 0:
                    S.op("dve", lambda R1=R1, R2=R2, R3=R3: nc.vector.tensor_tensor_scan(
                        out=R2, data0=R1[:, 0:HL], data1=R3, initial=0.0, op0=ALU.mult, op1=ALU.add), [b1, b3], [b2], cost=1.3)
                else:
                    S.op("dve", lambda R1=R1, R2=R2, R3=R3, pR2=pR2: nc.vector.tensor_tensor_scan(
                        out=R2, data0=R1[:, 0:HL], data1=R3, initial=pR2[:, HL - 1:HL], op0=ALU.mult, op1=ALU.add),
                        [b1, b3, pb2], [b2], cost=1.3)
                for t2 in range(1):
                    tb = hf
                    ps, pb = bank()
                    for kc in range(8):
                        mm(ps, wyr[i][:, kc, :], hT[:, kc, tb * 512:(tb + 1) * 512], kc == 0, kc == 7, [B_wyr[i], B_hT[tb]], [pb])
                    cp("act", Y1[:, t2 * 512:(t2 + 1) * 512], ps, [pb], [by1])
                    act(Y2[:, t2 * 512:(t2 + 1) * 512], ps, AF.Square, [pb], [by2])
                ts("dve", Y2, Y2, 0.044715, 1.0, ALU.mult, ALU.add, [by2], [by2])
                tt("pool", Y2, Y2, Y1, ALU.mult, [by2, by1], [by2])
                act(Y2, Y2, AF.Sigmoid, [by2], [by2], scale=1.5957691216057308)
                tt("pool", Y2, Y2, Y1, ALU.mult, [by2, by1], [by2])
                tt("dve", lruT[:, c, hf * HL:(hf + 1) * HL], Y2, R2, ALU.mult, [by2, b2], [B_lru[c]])
        if b == 0:
            dump("lruT", lruT, [128, 8, SEQ], BF16, B_lru)
        S.tag = "m"
        release(mB)
        if stage < 4:
            release(mseq)
            return

        mix_off = mark()
        mixedT = carve([128, 8, SEQ], BF16); B_mix = [Buf("mix%d" % i) for i in range(4)]
        mD1 = mark()
        wA = [carve([128, 4, 128], BF16) for _ in range(2)]; B_wA = [Buf(), Buf()]
        wB = [carve([128, 8, 128], BF16) for _ in range(2)]; B_wB = [Buf(), Buf()]
        wgA = [carve([128, 8, 128], BF16) for _ in range(2)]; B_wgA = [Buf(), Buf()]
        wgB = [carve([128, 8, 128], BF16) for _ in range(2)]; B_wgB = [Buf(), Buf()]
        sg = [carve([128, 512], F32) for _ in range(4)]; B_sg = [Buf() for _ in range(4)]
        tm = [carve([128, 512], F32) for _ in range(4)]; B_tm = [Buf() for _ in range(4)]
        kk = 0
        for oc in range(8):
            i = oc % 2
            if b == 0:
                for zb in range(12):
                    blk_ = oc * 12 + zb
                    dma("sp", xp_d[blk_ * 128:(blk_ + 1) * 128, :].rearrange("r (a n) -> r a n", a=8), zeros_b8, [B_const], [])
            dma("pool", wA[i], w_attn_o[:, oc * 128:(oc + 1) * 128].rearrange("(c p) n -> p c n", p=128), [], [B_wA[i]])
            dma("pool", wB[i], w_lru_o[:, oc * 128:(oc + 1) * 128].rearrange("(c p) n -> p c n", p=128), [], [B_wB[i]])
            dma("pool", wgA[i], wcols(6656 + oc * 128), [], [B_wgA[i]])
            dma("pool", wgB[i], wcols(7680 + oc * 128), [], [B_wgB[i]])
            for tb in range(4):
                tsl = slice(tb * 512, (tb + 1) * 512)
                pA, pAb = bank()
                for kc in range(4):
                    mm(pA, wA[i][:, kc, :], attnT[:, kc, tsl], kc == 0, kc == 3, [B_wA[i], B_attn[kc]], [pAb])
                pB, pBb = bank()
                for kc in range(8):
                    mm(pB, wB[i][:, kc, :], lruT[:, kc, tsl], kc == 0, kc == 7, [B_wB[i], B_lru[kc]], [pBb])
                pGA, pGAb = bank()
                for kc in range(8):
                    mm(pGA, wgA[i][:, kc, :], hT[:, kc, tsl], kc == 0, kc == 7, [B_wgA[i], B_hT[tb]], [pGAb])
                pGB, pGBb = bank()
                for kc in range(8):
                    mm(pGB, wgB[i][:, kc, :], hT[:, kc, tsl], kc == 0, kc == 7, [B_wgB[i], B_hT[tb]], [pGBb])
                j0 = kk % 4; j1 = (kk + 1) % 4; kk += 2
                act(sg[j0], pGA, AF.Sigmoid, [pGAb], [B_sg[j0]])
                act(sg[j1], pGB, AF.Sigmoid, [pGBb], [B_sg[j1]])
                tt("dve", tm[j0], pA, sg[j0], ALU.mult, [pAb, B_sg[j0]], [B_tm[j0]])
                tt("dve", tm[j1], pB, sg[j1], ALU.mult, [pBb, B_sg[j1]], [B_tm[j1]])
                tt("pool", mixedT[:, oc, tsl], tm[j0], tm[j1], ALU.add, [B_tm[j0], B_tm[j1]], [B_mix[tb]])
        if b == 0:
            dump("mixedT", mixedT, [128, 8, SEQ], BF16, B_mix)
        release(mseq)
        phase_d2(b, mixedT, B_mix)
        assert K.d2_top <= mix_off, (K.d2_top, mix_off)
        release(mseq)

    def phase_d2(b, mixedT, B_mix):
        mD2 = mark()
        g1f_rep = carve([128, D], F32); B_g1f = Buf()
        A2_rep = carve([128, D], F32); B_A2r = Buf()
        B2_rep = carve([128, D], F32); B_B2r = Buf()
        replicate(g1f_rep, B_g1f, lambda c: der[:, 2, c, b:b + 1])
        replicate(A2_rep, B_A2r, lambda c: der[:, 1, c, b:b + 1])
        replicate(B2_rep, B_B2r, lambda c: modT[:, 24 + c, b:b + 1])
        wout = carve([128, 8, D], BF16); B_wout = Buf()
        xt = [carve([128, D], F32) for _ in range(2)]; B_xt = [Buf(), Buf()]
        x1 = [carve([128, D], F32) for _ in range(2)]; B_x1 = [Buf(), Buf()]
        xn2_ = [carve([128, D], F32) for _ in range(2)]; B_xn2_ = [Buf(), Buf()]
        h2f_0 = carve([128, D], F32); h2f_ = [h2f_0, h2f_0]; B_h2f_0 = Buf(); B_h2f_ = [B_h2f_0, B_h2f_0]
        h2b = [carve([128, D], BF16) for _ in range(2)]; B_h2b = [Buf(), Buf()]
        h2T_ = [carve([128, 8, 128], F32) for _ in range(2)]; B_h2T_ = [Buf(), Buf()]
        junk = carve([128, D], BF16); B_junk = Buf()
        sq = [carve([128, 4], F32) for _ in range(2)]; B_sq = [Buf(), Buf()]
        lg_all = carve([128, NT, 36], F32); B_lgall = Buf()
        dma("pool", wout, w_out.rearrange("(c p) n -> p c n", p=128), [], [B_wout])
        for ti in range(NT):
            T = b * NT + ti
            i = ti % 2
            xn2 = xn2_[i]; B_xn2 = B_xn2_[i]; h2f = h2f_[i]; B_h2f = B_h2f_[i]
            h2T = h2T_[i]; B_h2T = B_h2T_[i]
            tsl = slice(ti * 128, (ti + 1) * 128)
            if ti == 0:
                dma("sp", xt[0], x[b, 0:128, :], [], [B_xt[0]])
            if ti + 1 < NT:
                dma("sp", xt[(ti + 1) % 2], x[b, (ti + 1) * 128:(ti + 2) * 128, :], [], [B_xt[(ti + 1) % 2]])
            for half in range(2):
                hs = slice(half * 512, (half + 1) * 512)
                ps, pb = bank()
                for kc in range(8):
                    mm(ps, mixedT[:, kc, tsl], wout[:, kc, hs], kc == 0, kc == 7, [B_mix[ti // 4], B_wout], [pb])
                tt("dve", x1[i][:, hs], ps, g1f_rep[:, hs], ALU.mult, [pb, B_g1f], [B_x1[i]])
            tt("pool", x1[i], x1[i], xt[i], ALU.add, [B_x1[i], B_xt[i]], [B_x1[i]])
            dma("sp", x1_d[T * 128:(T + 1) * 128, :], x1[i], [B_x1[i]], [])
            rms_stats(x1[i], B_x1[i], junk, B_junk, sq[i], B_sq[i])
            act(xn2, x1[i], AF.Copy, [B_x1[i], B_sq[i]], [B_xn2], scale=sq[i][:, 2:3])
            tt("dve", h2f, xn2, A2_rep, ALU.mult, [B_xn2, B_A2r], [B_h2f])
            tt("pool", h2b[i], h2f, B2_rep, ALU.add, [B_h2f, B_B2r], [B_h2b[i]])
            dma("sp", h2_d[T * 128:(T + 1) * 128, :], h2b[i], [B_h2b[i]], [])
            for half in range(2):
                ps, pb = bank()
                for c4 in range(4):
                    c = half * 4 + c4
                    tr(ps[:, c4 * 128:(c4 + 1) * 128], xn2[:, c * 128:(c + 1) * 128], ident_f, [B_xn2, B_const], [pb])
                for c4 in range(4):
                    c = half * 4 + c4
                    act(h2T[:, c, :], ps[:, c4 * 128:(c4 + 1) * 128], AF.Identity, [pb, B_mod], [B_h2T],
                        bias=modT[:, 24 + c, b:b + 1], scale=der[:, 1, c, b:b + 1])
            ps, pb = bank()
            for c in range(8):
                mm(ps[:, 0:36], h2T[:, c, :], wr_sb[:, c, :], c == 0, c == 7, [B_h2T, B_wr], [pb])
            tt("dve", lg_all[:, ti, :], ps[:, 0:36], br_sb, ALU.add, [pb, B_wr], [B_lgall])
        routing_seq(b, lg_all, B_lgall)
        if b == 0:
            dump("gw", gw_all, [128, 2, TT], F32, B_rt)
            dump("A0", A0_all, [128, TT, 32], BF16, B_rt)
            dump("A1", A1_all, [128, TT, 32], BF16, B_rt)
        K.d2_top = max(mark(), K.rt_top)
        release(mD2)

    def routing_seq(b, L, B_L):
        mR = mark()
        n = NT
        T0 = b * NT
        B_s = Buf("rscratch")
        R = [B_L, B_s]; W = [B_s]
        gl = L[:, :, 0:4]
        el = L[:, :, 4:36].rearrange("p t (g j) -> p t g j", g=4)
        gmax = carve([128, n], F32); goh = carve([128, n, 4], F32); gex = carve([128, n, 4], F32)
        gsum = carve([128, n], F32); ggate = carve([128, n], F32)
        tmp4 = carve([128, n, 4, 8], F32); ing = carve([128, n, 8], F32); msk = carve([128, n, 8], F32)
        m1 = carve([128, n], F32); m2 = carve([128, n], F32); oh0 = carve([128, n, 8], F32); oh1 = carve([128, n, 8], F32)
        e1 = carve([128, n], F32); rr_ = carve([128, n], F32)

        def bc3(a, k):
            return a.unsqueeze(2).to_broadcast([128, n, k])

        def red(out_, in_, op):
            S.op("dve", lambda: nc.vector.tensor_reduce(out=out_, in_=in_, axis=AX.X, op=op), R, W)

        red(gmax, gl, ALU.max)
        tt("dve", goh, gl, bc3(gmax, 4), ALU.is_equal, R, W)
        tt("dve", gex, gl, bc3(gmax, 4), ALU.subtract, R, W)
        act(gex, gex, AF.Exp, R, W)
        red(gsum, gex, ALU.add)
        S.op("dve", lambda: nc.vector.reciprocal(out=ggate, in_=gsum), R, W)
        tt("dve", tmp4, el, goh.unsqueeze(3).to_broadcast([128, n, 4, 8]), ALU.mult, R, W)
        red(ing, tmp4.rearrange("p t g j -> p t j g"), ALU.add)
        red(m1, ing, ALU.max)
        tt("dve", oh0, ing, bc3(m1, 8), ALU.is_equal, R, W)
        stt(msk, oh0, -1.0e30, ing, ALU.mult, ALU.add, R, W)
        red(m2, msk, ALU.max)
        tt("dve", oh1, msk, bc3(m2, 8), ALU.is_equal, R, W)
        tt("dve", e1, m2, m1, ALU.subtract, R, W)
        act(e1, e1, AF.Exp, R, W)
        ts("dve", rr_, e1, 1.0, None, ALU.add, None, R, W)
        S.op("dve", lambda: nc.vector.reciprocal(out=rr_, in_=rr_), R, W)
        Brt = [B_rt[T0 + t] for t in range(n)]
        W2 = [B_s] + Brt
        tt("dve", gw_all[:, 0, T0:T0 + n], rr_, ggate, ALU.mult, R, W2)
        tt("dve", e1, e1, rr_, ALU.mult, R, W)
        tt("dve", gw_all[:, 1, T0:T0 + n], e1, ggate, ALU.mult, R, W2)
        A0v = A0_all[:, T0:T0 + n, :].rearrange("p t (g j) -> p t g j", g=4)
        A1v = A1_all[:, T0:T0 + n, :].rearrange("p t (g j) -> p t g j", g=4)
        gohb = goh.unsqueeze(3).to_broadcast([128, n, 4, 8])
        tt("dve", A0v, oh0.unsqueeze(2).to_broadcast([128, n, 4, 8]), gohb, ALU.mult, R, W2)
        tt("dve", A1v, oh1.unsqueeze(2).to_broadcast([128, n, 4, 8]), gohb, ALU.mult, R, W2)
        tt("dve", As_all[:, T0:T0 + n, :], A0_all[:, T0:T0 + n, :], A1_all[:, T0:T0 + n, :], ALU.add, R + Brt, W2)
        K.rt_top = mark()
        release(mR)

    def phase_moe():
        issue_conv(len(conv_jobs))
        mM = mark()
        sz = carve([128, 32], F32); szi = carve([128, 32], I32); padf = carve([128, 32], F32)
        pend = carve([128, 32], F32); pstart = carve([128, 32], F32); ones32 = carve([128, 32], F32)
        B_sz = Buf()
        val = carve([128, 32], F32); tmpv = carve([128, 32], F32); dsum = carve([128, 2], F32); B_val = Buf()
        thr = carve([128, NBLK], F32); cmp3 = carve([128, NBLK, 32], F32); be_f = carve([128, NBLK], F32)
        idx_blk = carve([128, NBLK], U32); B_be = Buf()
        thr_i = carve([128, NBLK], I32)
        ps, pb = bank()
        for T in range(TT):
            mm(ps[:, 0:32], ones_b, As_all[:, T, :], T == 0, T == TT - 1, [B_const, B_rt[T]], [pb])
        ts("dve", szi, ps[:, 0:32], 127.0, None, ALU.add, None, [pb], [B_sz])
        S.op("dve", lambda: nc.vector.tensor_scalar(out=szi, in0=szi, scalar1=7, scalar2=7, op0=ALU.logical_shift_right,
                                                    op1=ALU.logical_shift_left), [B_sz], [B_sz])
        cp("dve", padf, szi, [B_sz], [B_sz])
        S.op("dve", lambda: nc.vector.memset(ones32, 1.0), [], [B_sz])
        S.op("dve", lambda: nc.vector.tensor_tensor_scan(out=pend, data0=ones32, data1=padf, initial=0.0,
                                                         op0=ALU.mult, op1=ALU.add), [B_sz], [B_sz])
        tt("dve", pstart, pend, padf, ALU.subtract, [B_sz], [B_sz])
        S.op("pool", lambda: nc.gpsimd.iota(thr_i, pattern=[[128, NBLK]], base=0, channel_multiplier=0), [], [B_be])
        cp("dve", thr, thr_i, [B_be], [B_be])
        tt("dve", cmp3, pend.unsqueeze(1).to_broadcast([128, NBLK, 32]), thr.unsqueeze(2).to_broadcast([128, NBLK, 32]),
           ALU.is_le, [B_sz, B_be], [B_be])
        S.op("dve", lambda: nc.vector.tensor_reduce(out=be_f, in_=cmp3, axis=AX.X, op=ALU.add), [B_be], [B_be])
        ts("dve", be_f, be_f, 31.0, 128.0, ALU.min, ALU.mult, [B_be], [B_be])
        skipm = carve([128, NBLK], F32)
        S.op("dve", lambda: nc.vector.memset(skipm, 0.0), [], [B_be])
        tt("dve", skipm[:, 1:NBLK], be_f[:, 1:NBLK], be_f[:, 0:NBLK - 1], ALU.is_equal, [B_be], [B_be])
        S.op("dve", lambda: nc.vector.memset(skipm[:, NBLK // 2:NBLK // 2 + 1], 0.0), [B_be], [B_be])
        stt(be_f, skipm, 1.0e6, be_f, ALU.mult, ALU.add, [B_be], [B_be])
        ts("dve", idx_blk, be_f, cst[:, 2:3], None, ALU.add, None, [B_be, B_const], [B_be])
        dump("pend", pend, [128, 32], F32, [B_sz])
        dump("idx_blk", idx_blk, [128, NBLK], U32, [B_be])
        h2l = [carve([128, D], BF16) for _ in range(6)]; B_h2l = [Buf() for _ in range(6)]
        B_xp = Buf("xp")
        for T in range(TT):
            ps, pb = bank()
            mm(ps[:, 0:32], ustrict, As_all[:, T, :], True, T == 0, [B_const, B_rt[T]], [pb])
            for T2 in range(T):
                mm(ps[:, 0:32], ones_b, As_all[:, T2, :], False, T2 == T - 1, [B_const, B_rt[T2]], [pb])
            tt("dve", val, ps[:, 0:32], pstart, ALU.add, [pb, B_sz], [B_val])
            for kx, A_ in enumerate((A0_all, A1_all)):
                tt("dve", tmpv, val, A_[:, T, :], ALU.mult, [B_val, B_rt[T]], [B_val])
                S.op("dve", lambda kx=kx: nc.vector.reduce_sum(out=dsum[:, kx:kx + 1], in_=tmpv, axis=AX.X), [B_val], [B_val])
                cp("dve", dest_all[:, kx, T:T + 1], dsum[:, kx:kx + 1], [B_val], [B_dest[T]])
            j = T % 6
            dma("sp", h2l[j], h2_d[T * 128:(T + 1) * 128, :], [], [B_h2l[j]])
            for kx in range(2):
                if "noscat" in dbg or ("scat1" in dbg and T >= 1):
                    continue
                scatter(xp_d, dest_all[:, kx, T:T + 1], h2l[j], [B_h2l[j], B_dest[T]], [])
        dump("dest", dest_all, [128, 2, TT], U32, B_dest)
        S.barrier()
        if stage < 6:
            release(mM)
            return
        mE = mark()
        w1f = [carve([128, 4096], BF16) for _ in range(2)]; B_w1 = [Buf(), Buf()]
        w3f = [carve([128, 4096], BF16) for _ in range(2)]; B_w3 = [Buf(), Buf()]
        w2f = [carve([128, 4096], BF16) for _ in range(2)]; B_w2 = [Buf(), Buf()]
        w1s = [t.rearrange("p (c n) -> p c n", c=8) for t in w1f]
        w3s = [t.rearrange("p (c n) -> p c n", c=8) for t in w3f]
        w2s = [t.rearrange("p (c n) -> p c n", c=4) for t in w2f]
        XB = 3
        xb = [carve([128, D], BF16) for _ in range(XB)]; B_xb = [Buf() for _ in range(XB)]
        xT = [carve([128, 8, 128], BF16) for _ in range(2)]; B_xT = [Buf(), Buf()]
        s1 = [carve([128, 512], F32) for _ in range(2)]; B_s1 = [Buf(), Buf()]
        gTt = [carve([128, 4, 128], BF16) for _ in range(2)]; B_gT = [Buf(), Buf()]
        ysb = [carve([128, D], F32) for _ in range(2)]; B_ys = [Buf(), Buf()]
        B_yp = Buf("yp")

        HB = NBLK // 2
        border = [(k // 2) if k % 2 == 0 else HB + k // 2 for k in range(NBLK)]

        def gather_w(out_, in_, idx, R, W):
            def fn():
                if getattr(K, "breg", None) is None:
                    K.breg = nc.gpsimd.to_reg(4095)
                return nc.gpsimd.indirect_dma_start(
                    out=out_, out_offset=None, in_=in_, in_offset=bass.IndirectOffsetOnAxis(ap=idx, axis=0),
                    bounds_check=K.breg, oob_is_err=False)
            return S.dma("pool", fn, R, W, xfer=9.0)

        def load_x(blk):
            j = blk % XB
            bid = border[blk]
            dma("sp", xb[j], xp_d[bid * 128:(bid + 1) * 128, :], [B_xp], [B_xb[j]])

        def load_w13(blk):
            i = blk % 2
            bid = border[blk]
            idx = idx_blk[:, bid:bid + 1]
            gather_w(w1f[i], w1b, idx, [B_be], [B_w1[i]])
            gather_w(w3f[i], w3b, idx, [B_be], [B_w3[i]])

        def load_w2(blk):
            i = blk % 2
            bid = border[blk]
            idx = idx_blk[:, bid:bid + 1]
            gather_w(w2f[i], w2b, idx, [B_be], [B_w2[i]])

        def stageA(blk):
            i = blk % 2
            j = blk % XB
            ps, pb = bank()
            psb = ps.bitcast(BF16)
            xbv = xb[j].rearrange("r (p c) -> r c p", c=8)
            for c in range(8):
                tr(psb[:, c * 128:(c + 1) * 128], xbv[:, c, :], ident_b, [B_xb[j], B_const], [pb])
            cp("dve", xT[i].rearrange("p c r -> p (c r)"), psb, [pb], [B_xT[i]])
            p1, p1b = bank()
            p3, p3b = bank()
            for (pp, ppb, ws, bws) in ((p1, p1b, w1s[i], B_w1[i]), (p3, p3b, w3s[i], B_w3[i])):
                wsv = ws.rearrange("p c (m j) -> p c j m", j=4)
                for jj in range(4):
                    for c in range(8):
                        mm(pp[:, jj * 128:(jj + 1) * 128], wsv[:, c, jj, :], xT[i][:, c, :], c == 0, c == 7, [bws, B_xT[i]], [ppb])
            act(s1[i], p1, AF.Sigmoid, [p1b], [B_s1[i]])
            tt("dve", s1[i], s1[i], p1, ALU.mult, [B_s1[i], p1b], [B_s1[i]])
            tt("dve", gTt[i].rearrange("p j r -> p (j r)"), s1[i], p3, ALU.mult, [B_s1[i], p3b], [B_gT[i]])

        def stageB(blk):
            i = blk % 2
            bid = border[blk]
            for half in range(2):
                py, pyb = bank()
                for jj in range(4):
                    mm(py, gTt[i][:, jj, :], w2s[i][:, jj, half * 512:(half + 1) * 512], jj == 0, jj == 3, [B_gT[i], B_w2[i]], [pyb])
                if half == 0:
                    cp("act", ysb[i][:, 0:512], py, [pyb], [B_ys[i]])
                else:
                    cp("dve", ysb[i][:, 512:1024], py, [pyb], [B_ys[i]])
            dma("sp", yp_d[bid * 128:(bid + 1) * 128, :], ysb[i], [B_ys[i]], [])

        load_x(0); load_x(1)
        load_w13(0); load_w13(1); load_w2(0); load_w2(1)
        for blk in range(NBLK + 1):
            if blk < NBLK:
                if blk + 2 < NBLK:
                    load_x(blk + 2)
                stageA(blk)
                if blk + 2 < NBLK:
                    load_w13(blk + 2)
            if blk >= 1:
                stageB(blk - 1)
                if 2 <= blk + 1 < NBLK:
                    load_w2(blk + 1)
        S.barrier()
        release(mE)
        if stage < 7:
            release(mM)
            return
        NB3 = 5
        g2f_rep = carve([128, NS, D], F32); B_g2f = Buf()
        gf_sb = carve([128, D], F32); B_gf = Buf()
        dma("sp", gf_sb, gf_rep, [], [B_gf])
        for b_ in range(NS):
            replicate(g2f_rep[:, b_, :], B_g2f, lambda c, b_=b_: der[:, 3, c, b_:b_ + 1])
        x1l = [carve([128, D], F32) for _ in range(NB3)]; B_x1l = [Buf() for _ in range(NB3)]
        y0 = [carve([128, D], F32) for _ in range(NB3)]; B_y0 = [Buf() for _ in range(NB3)]
        y1 = [carve([128, D], F32) for _ in range(NB3)]; B_y1 = [Buf() for _ in range(NB3)]
        ot = [carve([128, D], F32) for _ in range(2)]; B_ot = [Buf(), Buf()]
        junk = carve([128, D], BF16); B_junk = Buf()
        sq = [carve([128, 4], F32) for _ in range(2)]; B_sq = [Buf(), Buf()]

        def comb_load(T):
            i = T % NB3
            dma("sp", x1l[i], x1_d[T * 128:(T + 1) * 128, :], [], [B_x1l[i]])
            gather(y0[i], yp_d, dest_all[:, 0, T:T + 1], [B_yp, B_dest[T]], [B_y0[i]])
            gather(y1[i], yp_d, dest_all[:, 1, T:T + 1], [B_yp, B_dest[T]], [B_y1[i]])

        comb_load(0); comb_load(1)
        for T in range(TT):
            b, ti = divmod(T, NT)
            i = T % NB3
            k2 = T % 2
            if T + 2 < TT:
                comb_load(T + 2)
            act(y0[i], y0[i], AF.Copy, [B_y0[i], B_rt[T]], [B_y0[i]], scale=gw_all[:, 0, T:T + 1])
            stt(y0[i], y1[i], gw_all[:, 1, T:T + 1], y0[i], ALU.mult, ALU.add, [B_y0[i], B_y1[i], B_rt[T]], [B_y0[i]])
            tt("dve", y0[i], y0[i], g2f_rep[:, b, :], ALU.mult, [B_y0[i], B_g2f], [B_y0[i]])
            tt("pool", y0[i], y0[i], x1l[i], ALU.add, [B_y0[i], B_x1l[i]], [B_y0[i]])
            rms_stats(y0[i], B_y0[i], junk, B_junk, sq[k2], B_sq[k2])
            act(y0[i], y0[i], AF.Copy, [B_y0[i], B_sq[k2]], [B_y0[i]], scale=sq[k2][:, 2:3])
            tt("dve", ot[k2], y0[i], gf_sb, ALU.mult, [B_y0[i], B_gf], [B_ot[k2]])
            dma("sp", out[b, ti * 128:(ti + 1) * 128, :], ot[k2], [B_ot[k2]], [])
        release(mM)


    K.last = None
    if stage >= 1:
        for b in range(NS):
            phase_seq(b)
    if stage >= 5:
        phase_moe()

    S.replay()
    return nc, dbg_out


def _fm(v):
    return np.ascontiguousarray(np.asarray(v).reshape(8, 128).T)


def _shared_inputs(inp):
    f = lambda a: np.ascontiguousarray(np.asarray(a, dtype=np.float32))
    sh = {}
    sh["w_mod"] = f(inp["w_mod"][0])
    sh["b_modT"] = f(np.asarray(inp["b_mod"][0]).reshape(48, 128).T)
    sh["gT"] = f(np.stack([_fm(inp["norm1_g"][0]), _fm(inp["norm2_g"][0])], axis=1))
    sh["gf_rep"] = f(np.broadcast_to(np.asarray(inp["norm_f_g"])[None, :], (128, D)))
    sh["w_in"] = f(inp["w_in"][0])
    sh["conv_wT"] = f(np.asarray(inp["conv_w"][0]).T.reshape(8, 128, 4).transpose(1, 0, 2))
    vecs = np.stack([np.asarray(inp["conv_b"][0]), np.asarray(inp["lru_bx"][0]),
                     np.asarray(inp["lru_ba"][0]), np.asarray(inp["lru_lambda"][0])], axis=0)
    sh["lru_vecs"] = f(vecs.reshape(4, 8, 128).transpose(2, 0, 1))
    for name, key in (("wx_blk", "lru_wx"), ("wa_blk", "lru_wa")):
        wsrc = np.asarray(inp[key][0])
        blk = np.zeros((8, 128, 128), np.float32)
        for n in range(16):
            o = (n % 2) * 64
            blk[n // 2, o:o + 64, o:o + 64] = wsrc[n]
        sh[name] = blk
    sh["w_attn_o"] = f(inp["w_attn_o"][0])
    sh["w_lru_o"] = f(inp["w_lru_o"][0])
    sh["w_out"] = f(inp["w_out"][0])
    wr_full = np.concatenate([np.asarray(inp["w_grp"][0]), np.asarray(inp["w_exp"][0])], axis=1)
    sh["wr"] = f(wr_full.reshape(8, 128, 36).transpose(1, 0, 2))
    br = np.concatenate([np.asarray(inp["b_grp"][0]), np.asarray(inp["b_exp"][0])], axis=0)
    sh["br_rep"] = f(np.broadcast_to(br[None, :], (128, 36)))
    sh["w1"] = f(np.asarray(inp["w1"][0]).reshape(4096, 4096))
    sh["w3"] = f(np.asarray(inp["w3"][0]).reshape(4096, 4096))
    sh["w2"] = f(np.asarray(inp["w2"][0]).reshape(4096, 4096))
    return sh


def _core_inputs(inp, sh, core):
    m = dict(sh)
    xs = np.asarray(inp["x"])[core * NS:(core + 1) * NS]
    m["x"] = np.ascontiguousarray(xs, dtype=np.float32)
    cs = np.asarray(inp["c"])[core * NS:(core + 1) * NS]
    m["cT"] = np.ascontiguousarray(cs.T.reshape(8, 128, NS).transpose(1, 0, 2), dtype=np.float32)
    return m


def kernel(**inputs):
    nc, _ = build_program()
    sh = _shared_inputs(inputs)
    in_maps = [_core_inputs(inputs, sh, core) for core in range(NCORES)]
    res = run_bass_kernel_spmd(nc, in_maps, core_ids=list(range(NCORES)))
    outs = [np.asarray(r["out"], dtype=np.float32) for r in res.results]
    return np.concatenate(outs, axis=0)
```

```python
import numpy as np
import concourse.bass as bass
import concourse.mybir as mybir
from concourse.bass_utils import run_bass_kernel_spmd

F32 = mybir.dt.float32
BF16 = mybir.dt.bfloat16
U32 = mybir.dt.uint32
I32 = mybir.dt.int32
U8 = mybir.dt.uint8
AF = mybir.ActivationFunctionType
ALU = mybir.AluOpType
AX = mybir.AxisListType

SEM_LIM = 8000
N_DMA_SLOTS = 16
PRIO_CRITICAL_PATH = True
PCP_W = 0.5
STRICT_SAME_ENGINE = True


class Buf:
    __slots__ = ("name", "w", "r")

    def __init__(self, name=""):
        self.name = name
        self.w = None
        self.r = []


class _Op:
    __slots__ = ("idx", "eng", "fn", "is_dma", "key", "nslots", "preds", "nsucc", "succs", "cost", "xfer",
                 "epoch", "pos", "ev", "ready", "npred", "finish", "rawsame", "tag", "prio", "tbl")

    def __init__(self):
        self.preds = set()
        self.succs = []
        self.rawsame = set()


class Sched:
    ENGS = ("pe", "dve", "act", "pool", "sp")

    def __init__(self, nc):
        self.nc = nc
        self.ops = []
        self.epoch = 0
        self.sems = []
        self.slots = {}
        self.final = None
        self.tag = "m"

    def _new_sem(self):
        h = self.nc.alloc_semaphore()
        self.sems.append(h)
        return len(self.sems) - 1

    def barrier(self):
        self.epoch += 1

    def _record(self, eng, fn, reads, writes, is_dma, key, nslots, cost, xfer, tbl=None):
        o = _Op()
        o.tbl = tbl
        o.idx = len(self.ops)
        o.eng = eng; o.fn = fn; o.is_dma = is_dma; o.key = key; o.nslots = nslots
        o.cost = cost; o.xfer = xfer; o.epoch = self.epoch; o.tag = self.tag
        for b in reads:
            if b.w is not None:
                o.preds.add(b.w)
                o.rawsame.add(b.w)
        for b in writes:
            if b.w is not None:
                o.preds.add(b.w)
            for r in b.r:
                o.preds.add(r)
        o.preds.discard(o.idx)
        self.ops.append(o)
        for b in writes:
            b.w = o.idx
            b.r = []
        for b in reads:
            b.r.append(o.idx)
        return o.idx

    def op(self, eng, fn, reads=(), writes=(), cost=0.3, tbl=None):
        return self._record(eng, fn, reads, writes, False, None, 0, cost, 0.0, tbl)

    def dma(self, eng, fn, reads=(), writes=(), pool=None, nslots=N_DMA_SLOTS, cost=None, xfer=4.0):
        key = eng if pool is None else eng + ":" + pool
        if cost is None:
            cost = 0.15 if eng == "sp" else 1.1
        return self._record(eng, fn, reads, writes, True, key, nslots, cost, xfer)

    def _schedule(self):
        import heapq
        ops = self.ops
        n = len(ops)
        ep_count = {}
        for o in ops:
            ep_count[o.epoch] = ep_count.get(o.epoch, 0) + 1
        for o in ops:
            o.npred = 0
            o.ready = 0.0
        for o in ops:
            keep = set()
            for p in o.preds:
                if ops[p].epoch == o.epoch:
                    keep.add(p)
            o.preds = keep
            o.npred = len(keep)
            for p in keep:
                ops[p].succs.append(o.idx)
        order = {e: [] for e in self.ENGS}
        free = {e: 0.0 for e in self.ENGS}
        cur_tbl = [None]
        skipped = [0]
        act_pick = [False]
        by_epoch = {}
        for o in ops:
            by_epoch.setdefault(o.epoch, []).append(o)
        tbase = 0.0
        for ep in sorted(by_epoch):
            eops = by_epoch[ep]
            for e in self.ENGS:
                free[e] = max(free[e], tbase)
            future = {e: [] for e in self.ENGS}
            now = {e: [] for e in self.ENGS}
            groups = {}
            for o in eops:
                groups.setdefault(o.tag, []).append(o)
            for g_ in groups.values():
                m_ = float(len(g_))
                for r_, o in enumerate(g_):
                    o.prio = r_ / m_
            if PRIO_CRITICAL_PATH:
                bl = {}
                for o in reversed(eops):
                    b_ = 0.0
                    for s_ in o.succs:
                        v_ = bl.get(s_, 0.0)
                        if v_ > b_:
                            b_ = v_
                    bl[o.idx] = b_ + o.cost + o.xfer
                mx_ = max(bl.values()) if bl else 1.0
                for o in eops:
                    o.prio = (1.0 - PCP_W) * o.prio + PCP_W * (1.0 - bl[o.idx] / mx_)
            for o in eops:
                if o.npred == 0:
                    o.ready = tbase
                    heapq.heappush(future[o.eng], (o.ready, o.prio, o.idx))
            left = len(eops)
            tmax = tbase
            while left:
                best = None
                for e in self.ENGS:
                    f = future[e]; nw = now[e]
                    while f and f[0][0] <= free[e]:
                        x_ = heapq.heappop(f)
                        heapq.heappush(nw, (x_[1], x_[2]))
                    if nw:
                        if e == "act" and len(nw) > 1:
                            head = nw[0]
                            ht = ops[head[1]].tbl
                            if ht is not None and ht != cur_tbl[0] and skipped[0] < 8:
                                pulled = [heapq.heappop(nw) for _ in range(min(10, len(nw)))]
                                pick = None
                                for it_ in pulled:
                                    t_ = ops[it_[1]].tbl
                                    if t_ is None or t_ == cur_tbl[0]:
                                        pick = it_
                                        break
                                for it_ in pulled:
                                    if it_ is not pick:
                                        heapq.heappush(nw, it_)
                                if pick is not None:
                                    heapq.heappush(nw, (-1.0, pick[1]))
                                    act_pick[0] = True
                        cand = (free[e], nw[0][0], nw[0][1], e, True)
                    elif f:
                        cand = (f[0][0], f[0][1], f[0][2], e, False)
                    else:
                        continue
                    if best is None or cand[:3] < best[:3]:
                        best = cand
                start, _pr, idx, e, from_now = best
                if from_now:
                    heapq.heappop(now[e])
                else:
                    heapq.heappop(future[e])
                o = ops[idx]
                if e == "act":
                    if act_pick[0] and from_now:
                        skipped[0] += 1
                    else:
                        skipped[0] = 0
                    if o.tbl is not None and o.tbl != cur_tbl[0]:
                        o.cost += 1.3
                        cur_tbl[0] = o.tbl
                act_pick[0] = False
                o.pos = len(order[e])
                order[e].append(o)
                free[e] = start + o.cost
                o.finish = start + o.cost + o.xfer
                tmax = max(tmax, o.finish)
                left -= 1
                for s in o.succs:
                    so = ops[s]
                    so.npred -= 1
                    if so.ready < o.finish:
                        so.ready = o.finish
                    if so.npred == 0:
                        heapq.heappush(future[so.eng], (so.ready, so.prio, so.idx))
            tbase = tmax
        self.est_us = tbase
        return order

    def replay(self):
        nc = self.nc
        ops = self.ops
        order = self._schedule()
        chains = {e: [] for e in self.ENGS}
        cnt = {e: 0 for e in self.ENGS}
        slots = {}
        rr = {}
        slot_wait = {}
        for e in self.ENGS:
            for o in order[e]:
                if o.is_dma:
                    sl = slots.setdefault(o.key, [])
                    if len(sl) < o.nslots:
                        sl.append([self._new_sem(), 0])
                        slot = sl[-1]
                    else:
                        k = rr.get(o.key, 0)
                        rr[o.key] = k + 1
                        slot = sl[k % o.nslots]
                        if (slot[1] + 1) * 16 > SEM_LIM * 4:
                            slot[0] = self._new_sem()
                            slot[1] = 0
                    if slot[1] > 0:
                        slot_wait[o.idx] = (slot[0], slot[1] * 16)
                    slot[1] += 1
                    o.ev = (slot[0], slot[1] * 16, 16)
                else:
                    j, v = divmod(cnt[e], SEM_LIM)
                    if j >= len(chains[e]):
                        chains[e].append(self._new_sem())
                    cnt[e] += 1
                    o.ev = (chains[e][j], v + 1, 1)
        self.slots = slots
        nep = self.epoch + 1
        ep_last = [dict() for _ in range(nep)]
        for o in ops:
            d = ep_last[o.epoch]
            s, v, _ = o.ev
            if d.get(s, 0) < v:
                d[s] = v
        cum = []
        acc = {}
        for k in range(nep):
            cum.append(dict(acc))
            for s, v in ep_last[k].items():
                if acc.get(s, 0) < v:
                    acc[s] = v
        self.final = acc
        sems = self.sems
        prog = {}
        for e in self.ENGS:
            own = set(chains[e])
            seen = {}
            lst = []
            cur_ep = -1
            for o in order[e]:
                deps = {}
                if o.epoch != cur_ep:
                    cur_ep = o.epoch
                    for s, v in cum[o.epoch].items():
                        if s not in own:
                            deps[s] = v
                for p in o.preds:
                    po = ops[p]
                    s, v, _ = po.ev
                    if s in own:
                        if e == "pe" or (p not in o.rawsame and not STRICT_SAME_ENGINE):
                            continue
                    if deps.get(s, 0) < v:
                        deps[s] = v
                if o.idx in slot_wait:
                    s, v = slot_wait[o.idx]
                    if deps.get(s, 0) < v:
                        deps[s] = v
                waits = []
                for s, v in deps.items():
                    if seen.get(s, 0) < v:
                        seen[s] = v
                        waits.append((s, v))
                lst.append((waits, o.fn, o.ev))
            prog[e] = (lst, own)
        final_waits = [(s, v) for s, v in self.final.items()]
        with nc.Block() as block:
            for e in self.ENGS:
                lst, own = prog[e]
                starter = {"pe": block.tensor, "dve": block.vector, "act": block.scalar,
                           "pool": block.gpsimd, "sp": block.sync}[e]

                def body(engine, lst=lst, e=e, own=own):
                    for waits, fn, (s, v, inc) in lst:
                        for ws, wv in waits:
                            engine.wait_ge(sems[ws], wv)
                        ins = fn()
                        ins.then_inc(sems[s], inc)
                    if e == "sp":
                        for ws, wv in final_waits:
                            if ws not in own:
                                engine.wait_ge(sems[ws], wv)
                starter(body)


NCORES = 8
NS = 2
SEQ = 2048
D = 1024
NT = SEQ // 128
TT = NS * NT
IN_COLS = 8704
NBLK = 96
EPS = 1e-6


class Ctx:
    pass


def build_program(stage=99, dbg=()):
    nc = bass.Bass("TRN2", target_bir_lowering=False)
    S = Sched(nc)
    K = Ctx()

    def din(name, shape, dt=F32):
        return nc.dram_tensor(name, list(shape), dt, kind="ExternalInput").ap()

    def dscr(name, shape, dt):
        return nc.dram_tensor(name, list(shape), dt, kind="Internal").ap()

    x = din("x", [NS, SEQ, D])
    cT = din("cT", [128, 8, NS])
    w_mod = din("w_mod", [D, 6 * D])
    b_modT = din("b_modT", [128, 48])
    gT = din("gT", [128, 2, 8])
    gf_rep = din("gf_rep", [128, D])
    w_in = din("w_in", [D, IN_COLS])
    conv_wT = din("conv_wT", [128, 8, 4])
    lru_vecs = din("lru_vecs", [128, 4, 8])
    wx_blk = din("wx_blk", [8, 128, 128])
    wa_blk = din("wa_blk", [8, 128, 128])
    w_attn_o = din("w_attn_o", [512, D])
    w_lru_o = din("w_lru_o", [D, D])
    w_out = din("w_out", [D, D])
    wr = din("wr", [128, 8, 36])
    br_rep = din("br_rep", [128, 36])
    w1 = din("w1", [4096, 4096])
    w3 = din("w3", [4096, 4096])
    w2 = din("w2", [4096, 4096])
    out = nc.dram_tensor("out", [NS, SEQ, D], F32, kind="ExternalOutput").ap()
    x1_d = dscr("x1_d", [TT * 128, D], F32)
    h2_d = dscr("h2_d", [TT * 128, D], BF16)
    xp_d = dscr("xp_d", [NBLK * 128, D], BF16)
    yp_d = dscr("yp_d", [NBLK * 128, D], F32)
    w1b = dscr("w1b", [4096, 4096], BF16)
    w3b = dscr("w3b", [4096, 4096], BF16)
    w2b = dscr("w2b", [4096, 4096], BF16)
    conv_jobs = []
    for e in range(32):
        for (dst_, src_) in ((w1b, w1), (w3b, w3), (w2b, w2)):
            conv_jobs.append((dst_[e * 128:(e + 1) * 128, :].rearrange("r (a n) -> r a n", a=2),
                              src_[e * 128:(e + 1) * 128, :].rearrange("r (a n) -> r a n", a=2)))

    def issue_conv(n, after=()):
        for _ in range(n):
            if not conv_jobs:
                return
            d_, s_ = conv_jobs.pop(0)
            S.dma("pool", lambda d_=d_, s_=s_: nc.gpsimd.dma_start(out=d_, in_=s_), list(after), [], pool="conv", nslots=16, xfer=20.0)
    dbg_out = {}

    TOT = 206 * 1024
    arena = nc.alloc_sbuf_tensor("arena", [128, TOT], U8).ap()
    st = {"off": 0}
    SZ = {F32: 4, BF16: 2, U32: 4, I32: 4}

    def carve(shape, dt):
        n = int(np.prod(shape[1:])) * SZ[dt]
        assert st["off"] + n <= TOT, ("SBUF overflow", st["off"], n)
        t = arena[:, st["off"]:st["off"] + n].bitcast(dt)
        st["off"] += (n + 63) // 64 * 64
        if len(shape) == 3:
            t = t.rearrange("p (a b) -> p a b", a=shape[1])
        elif len(shape) == 4:
            t = t.rearrange("p (a b c) -> p a b c", a=shape[1], b=shape[2])
        return t

    def mark():
        return st["off"]

    def release(m):
        S.barrier()
        st["off"] = m

    banks = [nc.alloc_psum_tensor("bank%d" % i, [128, 512], F32).ap() for i in range(8)]
    bank_b = [Buf("bank%d" % i) for i in range(8)]
    bk = {"i": 0, "o": 0, "c": 0}

    def bank():
        if S.tag == "B":
            i = 2 + bk["i"] % 4
            bk["i"] += 1
        elif S.tag == "C":
            i = 6 + bk["c"] % 2
            bk["c"] += 1
        else:
            i = 2 + bk["i"] % 6
            bk["i"] += 1
        return banks[i], bank_b[i]

    def bank_o():
        i = bk["o"] % 2
        bk["o"] += 1
        return banks[i], bank_b[i]

    DSZ = {F32: 4, BF16: 2, U32: 4, I32: 4, U8: 1}

    def fsz(ap):
        n = 1
        for s_ in ap.shape[1:]:
            n *= int(s_)
        return n

    def act(out_, in_, func, R, W, bias=None, scale=None, accum=None):
        kw = {}
        if bias is not None:
            kw["bias"] = bias
        if scale is not None:
            kw["scale"] = scale
        if accum is not None:
            kw["accum_out"] = accum
        tbl = {AF.Exp: "e", AF.Ln: "e", AF.Sigmoid: "s", AF.Sqrt: "q"}.get(func)
        S.op("act", lambda: nc.scalar.activation(out=out_, in_=in_, func=func, **kw), R, W,
             cost=0.2 + fsz(out_) / 1000.0, tbl=tbl)

    def ecost(eng, n, slow=1.0):
        if eng == "pool":
            return 0.25 + n / 435.0
        return 0.12 + slow * n / 1250.0

    def tt(eng, out_, in0, in1, op, R, W):
        e = nc.vector if eng == "dve" else nc.gpsimd
        S.op(eng, lambda: e.tensor_tensor(out=out_, in0=in0, in1=in1, op=op), R, W, cost=ecost(eng, fsz(out_), 1.55))

    def ts(eng, out_, in0, s1, s2, op0, op1, R, W):
        e = nc.vector if eng == "dve" else nc.gpsimd
        c_ = ecost(eng, fsz(out_))
        if op1 is None:
            S.op(eng, lambda: e.tensor_scalar(out=out_, in0=in0, scalar1=s1, scalar2=None, op0=op0), R, W, cost=c_)
        else:
            S.op(eng, lambda: e.tensor_scalar(out=out_, in0=in0, scalar1=s1, scalar2=s2, op0=op0, op1=op1), R, W, cost=c_)

    def stt(out_, in0, scalar, in1, op0, op1, R, W):
        S.op("dve", lambda: nc.vector.scalar_tensor_tensor(out=out_, in0=in0, scalar=scalar, in1=in1,
                                                           op0=op0, op1=op1), R, W, cost=ecost("dve", fsz(out_), 1.55))

    def cp(eng, out_, in_, R, W):
        if eng == "act":
            S.op("act", lambda: nc.scalar.copy(out=out_, in_=in_), R, W, cost=0.2 + fsz(out_) / 1000.0)
        else:
            e = nc.vector if eng == "dve" else nc.gpsimd
            S.op(eng, lambda: e.tensor_copy(out=out_, in_=in_), R, W, cost=ecost(eng, fsz(out_)))

    def mm(out_, lhsT, rhs, start, stop, R, W):
        S.op("pe", lambda: nc.tensor.matmul(out_, lhsT=lhsT, rhs=rhs, start=start, stop=stop), R, W,
             cost=max(0.065, fsz(rhs) / 1400.0) * (4.0 if rhs.dtype == F32 else 1.0))

    def tr(out_, in_, ident, R, W):
        S.op("pe", lambda: nc.tensor.transpose(out=out_, in_=in_, identity=ident), R, W, cost=0.1)

    def dma(q, out_, in_, R, W):
        e = nc.sync if q == "sp" else nc.gpsimd
        nbytes = fsz(out_) * int(out_.shape[0]) * DSZ[out_.dtype]
        return S.dma(q, lambda: e.dma_start(out=out_, in_=in_), R, W, xfer=1.5 + nbytes / 250e3)

    def gather(out_, in_, idx, R, W):
        nbytes = fsz(out_) * int(out_.shape[0]) * DSZ[out_.dtype]
        return S.dma("pool", lambda: nc.gpsimd.indirect_dma_start(
            out=out_, out_offset=None, in_=in_,
            in_offset=bass.IndirectOffsetOnAxis(ap=idx, axis=0)), R, W, xfer=1.5 + nbytes / 250e3)

    def scatter(out_, idx, in_, R, W):
        nbytes = fsz(in_) * int(in_.shape[0]) * DSZ[in_.dtype]
        return S.dma("pool", lambda: nc.gpsimd.indirect_dma_start(
            out=out_, out_offset=bass.IndirectOffsetOnAxis(ap=idx, axis=0), in_=in_,
            in_offset=None), R, W, xfer=1.5 + nbytes / 250e3)

    def dump(name, sb_ap, shape, dt, R):
        if name not in dbg:
            return
        d = nc.dram_tensor("dbg_" + name, list(shape), dt, kind="ExternalOutput").ap()
        dbg_out[name] = d
        dma("sp", d, sb_ap, R, [])

    ident_f = carve([128, 128], F32); B_const = Buf("const")
    ident_b = carve([128, 128], BF16)
    iota_i = carve([128, 128], I32)
    mask = carve([128, 256], BF16)
    ustrict = carve([128, 128], BF16)
    ones_b = carve([128, 128], BF16)
    zeros_f = carve([128, 128], F32)
    zeros_b8 = zeros_f.bitcast(BF16).rearrange("p (a n) -> p a n", a=2)[:, 0:1, :].to_broadcast([128, 8, 128])
    cst = carve([128, 4], F32)
    modT = carve([128, 48, NS], F32); B_mod = Buf("mod")
    B_mod1 = Buf("mod1")
    B_modc = Buf("modc")
    gT_sb = carve([128, 2, 8], F32)
    bmod_sb = carve([128, 48], F32)
    der = carve([128, 6, 8, NS], F32)
    convw_sb = carve([128, 8, 4], F32)
    lruv_sb = carve([128, 4, 8], F32)
    cl_sb = carve([128, 3, 8], F32)
    wxb_sb = carve([128, 8, 128], BF16)
    wab_sb = carve([128, 8, 128], BF16)
    B_lruc = Buf("lruconst")
    wr_sb = carve([128, 8, 36], F32)
    br_sb = carve([128, 36], F32)
    B_wr = Buf()
    A0_all = carve([128, TT, 32], BF16)
    A1_all = carve([128, TT, 32], BF16)
    As_all = carve([128, TT, 32], BF16)
    gw_all = carve([128, 2, TT], F32)
    dest_all = carve([128, 2, TT], U32)
    B_rt = [Buf("rt%d" % t) for t in range(TT)]
    B_dest = [Buf("dest%d" % t) for t in range(TT)]

    S.op("pool", lambda: nc.gpsimd.iota(iota_i, pattern=[[1, 128]], base=0, channel_multiplier=-1), [], [B_const])
    ts("dve", ident_f, iota_i, 0, None, ALU.is_equal, None, [B_const], [B_const])
    ts("dve", ident_b, iota_i, 0, None, ALU.is_equal, None, [B_const], [B_const])
    ts("dve", mask[:, 0:128], iota_i, 0, None, ALU.is_ge, None, [B_const], [B_const])
    ts("dve", mask[:, 128:256], iota_i, 0, None, ALU.is_le, None, [B_const], [B_const])
    ts("dve", ustrict, iota_i, 0, None, ALU.is_gt, None, [B_const], [B_const])
    S.op("dve", lambda: nc.vector.memset(ones_b, 1.0), [], [B_const])
    S.op("dve", lambda: nc.vector.memset(zeros_f, 0.0), [], [B_const])
    S.op("dve", lambda: nc.vector.memset(cst[:, 0:1], EPS), [], [B_const])
    S.op("dve", lambda: nc.vector.memset(cst[:, 1:2], 1.0), [], [B_const])
    ts("dve", cst[:, 2:3], iota_i[:, 0:1], -1.0, None, ALU.mult, None, [B_const], [B_const])
    eps_ap = cst[:, 0:1]
    one_ap = cst[:, 1:2]

    dma("sp", gT_sb, gT, [], [B_modc])
    dma("sp", bmod_sb, b_modT, [], [B_modc])
    dma("sp", convw_sb, conv_wT, [], [B_lruc])
    dma("sp", lruv_sb, lru_vecs, [], [B_lruc])
    dma("sp", wr_sb, wr, [], [B_wr])
    dma("sp", br_sb, br_rep, [], [B_wr])
    dma("pool", wxb_sb, wx_blk.rearrange("c i o -> i c o"), [], [B_lruc])
    dma("pool", wab_sb, wa_blk.rearrange("c i o -> i c o"), [], [B_lruc])
    act(cl_sb[:, 0, :], lruv_sb[:, 3, :], AF.Exp, [B_lruc], [B_lruc], scale=-1.0)
    act(cl_sb[:, 0, :], cl_sb[:, 0, :], AF.Ln, [B_lruc, B_const], [B_lruc], bias=one_ap)
    ts("dve", cl_sb[:, 1, :], cl_sb[:, 0, :], -8.0, None, ALU.mult, None, [B_lruc], [B_lruc])
    ts("dve", cl_sb[:, 2, :], cl_sb[:, 0, :], -16.0, None, ALU.mult, None, [B_lruc], [B_lruc])

    m0 = mark()
    st["off"] = 176 * 1024
    S.tag = "C"
    cact = carve([128, 8, NS], F32); B_cact = Buf()
    csig = carve([128, 8, NS], F32)
    dma("sp", cact, cT, [], [B_cact])
    act(csig, cact, AF.Sigmoid, [B_cact], [B_cact])
    tt("dve", cact, cact, csig, ALU.mult, [B_cact], [B_cact])
    cact_b = carve([128, 8, NS], BF16)
    cp("dve", cact_b, cact, [B_cact], [B_cact])
    wm = [carve([128, 8, 512], BF16) for _ in range(3)]
    B_wm = [Buf(), Buf(), Buf()]
    for blk in range(12):
        w_ = wm[blk % 3]; bw = B_wm[blk % 3]
        dma("pool", w_, w_mod[:, blk * 512:(blk + 1) * 512].rearrange("(c p) n -> p c n", p=128), [], [bw])
        ps, pb = bank()
        for o4 in range(4):
            for kc in range(8):
                mm(ps[:, o4 * 2:o4 * 2 + 2], w_[:, kc, o4 * 128:(o4 + 1) * 128], cact_b[:, kc, :],
                   kc == 0, kc == 7, [bw, B_cact], [pb])
        for o4 in range(4):
            oc = blk * 4 + o4
            ts("dve", modT[:, oc, :], ps[:, o4 * 2:o4 * 2 + 2], bmod_sb[:, oc:oc + 1], None, ALU.add, None,
               [pb, B_modc], [B_mod1 if oc < 16 else B_mod])
    for b in range(NS):
        stt(der[:, 0, :, b], modT[:, 8:16, b], 1.0, gT_sb[:, 0, :], ALU.add, ALU.mult, [B_mod1, B_modc], [B_mod1])
        stt(der[:, 1, :, b], modT[:, 32:40, b], 1.0, gT_sb[:, 1, :], ALU.add, ALU.mult, [B_mod, B_modc], [B_mod])
        ts("dve", der[:, 2, :, b], modT[:, 16:24, b], 1.0, None, ALU.add, None, [B_mod], [B_mod])
        ts("dve", der[:, 3, :, b], modT[:, 40:48, b], 1.0, None, ALU.add, None, [B_mod], [B_mod])
    dump("modT", modT, [128, 48, NS], F32, [B_mod, B_mod1])
    st["off"] = m0
    S.tag = "m"

    bc_t = [carve([128, 128], F32) for _ in range(2)]
    B_bc = [Buf(), Buf()]
    rr = {"i": 0}

    def replicate(dst, dst_b, vec_ap_fn):
        for half in range(2):
            ps, pb = bank()
            for c4 in range(4):
                c = half * 4 + c4
                i = rr["i"] % 2; rr["i"] += 1
                act(bc_t[i], zeros_f, AF.Identity, [B_const, B_mod], [B_bc[i]], bias=vec_ap_fn(c))
                mm(ps[:, c4 * 128:(c4 + 1) * 128], bc_t[i], ident_f, True, True, [B_bc[i], B_const], [pb])
            cp("dve", dst[:, half * 512:(half + 1) * 512], ps, [pb], [dst_b])

    def tokview(X, g):
        if g == 0:
            return X.rearrange("d (t j) -> d t j", j=128)
        if g == 1:
            return X.rearrange("d (n j r) -> d r n j", n=4, j=128, r=4)
        return X.rearrange("d (j r) -> d r j", r=16)

    def tile_ap(X, g, t):
        v = tokview(X, g)
        if g == 1:
            return v[:, t // 4, t % 4, :]
        return v[:, t, :]

    def pair_ap(X, g, t):
        v = tokview(X, g)
        if g == 0:
            return v[:, t:t + 2, :]
        return v[:, t // 4, (t % 4):(t % 4) + 2, :]

    def unit_ap(X, g, u):
        v = tokview(X, g)
        if g == 1:
            return v[:, u, :, :]
        return v[:, 4 * u:4 * u + 4, :]

    def has_next(g, t):
        return (g == 0 and t < 15) or (g == 1 and t % 4 < 3)

    def has_prev(g, t):
        return (g == 0 and t > 0) or (g == 1 and t % 4 > 0)

    def wcols(col0, n=128):
        return w_in[:, col0:col0 + n].rearrange("(c p) n -> p c n", p=128)

    def rms_stats(src, bsrc, junk, bjunk, sq, bsq):
        act(junk, src, AF.Square, [bsrc], [bjunk, bsq], accum=sq[:, 0:1])
        act(sq[:, 1:2], sq[:, 0:1], AF.Ln, [bsq, B_const], [bsq], bias=eps_ap, scale=1.0 / D)
        act(sq[:, 2:3], sq[:, 1:2], AF.Exp, [bsq], [bsq], scale=-0.5)

    def phase_seq(b):
        mseq = mark()
        hT = carve([128, 8, SEQ], BF16); B_hT = [Buf("hT%d" % i) for i in range(4)]
        attnT = carve([128, 4, SEQ], BF16); B_attn = [Buf("attn%d" % i) for i in range(4)]
        lruT = carve([128, 8, SEQ], BF16); B_lru = [Buf("lru%d" % i) for i in range(8)]

        mA = mark()
        S.tag = "B"
        xt = [carve([128, D], F32) for _ in range(2)]; B_xt = [Buf(), Buf()]
        xn = [carve([128, 4, D], F32) for _ in range(2)]; B_xn = [Buf(), Buf()]
        junk = carve([128, D], BF16); B_junk = Buf()
        sq = [carve([128, 4], F32) for _ in range(4)]; B_sq = [Buf() for _ in range(4)]
        k = 0
        for grp in range(4):
            xg = xn[grp % 2]; bxg = B_xn[grp % 2]
            for i4 in range(4):
                ti = grp * 4 + i4
                xti = xt[k % 2]; bxt = B_xt[k % 2]
                sqk = sq[k % 4]; bsq = B_sq[k % 4]
                k += 1
                dma("sp", xti, x[b, ti * 128:(ti + 1) * 128, :], [], [bxt])
                rms_stats(xti, bxt, junk, B_junk, sqk, bsq)
                ts("dve", xg[:, i4, :], xti, sqk[:, 2:3], None, ALU.mult, None, [bxt, bsq], [bxg])
            for c in range(8):
                ps, pb = bank()
                for i4 in range(4):
                    tr(ps[:, i4 * 128:(i4 + 1) * 128], xg[:, i4, c * 128:(c + 1) * 128], ident_f, [bxg, B_const], [pb])
                act(hT[:, c, grp * 512:(grp + 1) * 512], ps, AF.Identity, [pb, B_mod1], [B_hT[grp]],
                    bias=modT[:, c, b:b + 1], scale=der[:, 0, c, b:b + 1])
        if b == 0:
            dump("hT", hT, [128, 8, SEQ], BF16, B_hT)
        S.tag = "m"
        release(mA)
        if stage < 2:
            release(mseq)
            return

        mB = mark()
        S.tag = "B"
        wq = [carve([128, 8, 128], BF16) for _ in range(2)]; B_wq = [Buf(), Buf()]
        wk = [carve([128, 8, 128], BF16) for _ in range(2)]; B_wk = [Buf(), Buf()]
        wv = [carve([128, 8, 128], BF16) for _ in range(2)]; B_wv = [Buf(), Buf()]
        qT2_ = carve([128, SEQ], BF16); qT2 = [qT2_, qT2_]; B_q_ = Buf(); B_q = [B_q_, B_q_]
        kT2_ = carve([128, SEQ], BF16); kT2 = [kT2_, kT2_]; B_k_ = Buf(); B_k = [B_k_, B_k_]
        V_sb = [carve([128, 16, 256], BF16) for _ in range(2)]; B_V = [Buf(), Buf()]
        NP = 6
        Pexp = [carve([128, 256], BF16) for _ in range(NP)]; B_pe = [Buf() for _ in range(NP)]
        Pm = [carve([128, 256], BF16) for _ in range(NP)]; B_pm = [Buf() for _ in range(NP)]
        acc = [carve([128, SEQ], F32) for _ in range(2)]; B_acc = [Buf(), Buf()]
        rtmp = carve([128, SEQ], F32); B_rtmp = Buf()
        for i in range(2):
            S.op("pool", lambda v=V_sb[i]: nc.gpsimd.memset(v[:, :, 64:192], 1.0), [], [B_V[i]])
        it = 0
        pk = [0]
        for sp in range(4):
            for g in range(3):
                col0 = g * 512 + sp * 128
                i = it % 2; it += 1
                dma("pool", wq[i], wcols(col0), [], [B_wq[i]])
                dma("pool", wk[i], wcols(1536 + col0), [], [B_wk[i]])
                dma("pool", wv[i], wcols(3072 + col0), [], [B_wv[i]])
                for (w_, bw, dst, bdst, sc) in ((wq[i], B_wq[i], qT2[i], B_q[i], 0.125),
                                                (wk[i], B_wk[i], kT2[i], B_k[i], 1.0)):
                    for tb in range(4):
                        ps, pb = bank()
                        for kc in range(8):
                            mm(ps, w_[:, kc, :], hT[:, kc, tb * 512:(tb + 1) * 512], kc == 0, kc == 7,
                               [bw, B_hT[tb]], [pb])
                        ts("dve", dst[:, tb * 512:(tb + 1) * 512], ps, sc, None, ALU.mult, None, [pb], [bdst])
                for u in range(4):
                    ps, pb = bank()
                    for t4 in range(4):
                        t = u * 4 + t4
                        for kc in range(8):
                            mm(ps[:, t4 * 128:(t4 + 1) * 128], tile_ap(hT[:, kc, :], g, t), wv[i][:, kc, :],
                               kc == 0, kc == 7, [B_wv[i]] + B_hT, [pb])
                    psv = ps.rearrange("p (a b) -> p a b", a=4)
                    cp("act", V_sb[i][:, u * 4:(u + 1) * 4, 0:64], psv[:, :, 0:64], [pb], [B_V[i]])
                    cp("dve", V_sb[i][:, u * 4:(u + 1) * 4, 192:256], psv[:, :, 64:128], [pb], [B_V[i]])

                items = [(hd, t) for hd in range(2) for t in range(16)]
                slot_of = {}
                obs = {}

                def stage1(hd, t, i=i, g=g):
                    rows = slice(hd * 64, hd * 64 + 64)
                    qh = qT2[i][rows, :]; kh = kT2[i][rows, :]
                    ps, pb = bank()
                    nx = has_next(g, t)
                    N = 256 if nx else 128
                    if nx:
                        mm(ps[:, 0:256].rearrange("p (a b) -> p a b", a=2), tile_ap(kh, g, t), pair_ap(qh, g, t),
                           True, True, [B_k[i], B_q[i]], [pb])
                    else:
                        mm(ps[:, 0:128], tile_ap(kh, g, t), tile_ap(qh, g, t), True, True, [B_k[i], B_q[i]], [pb])
                    j = pk[0] % NP; pk[0] += 1
                    slot_of[(hd, t)] = j
                    act(Pexp[j][:, 0:N], ps[:, 0:N], AF.Exp, [pb], [B_pe[j]])
                    tt("dve", Pm[j][:, 0:N], Pexp[j][:, 0:N], mask[:, 0:N], ALU.mult, [B_pe[j], B_const], [B_pm[j]])

                def stage2(hd, t, i=i, g=g):
                    u, t4 = divmod(t, 4)
                    if t4 == 0:
                        obs[hd] = bank_o()
                    ob, obb = obs[hd]
                    j = slot_of[(hd, t)]
                    lv = V_sb[i][:, t, hd * 128:(hd + 1) * 128]
                    oc_ = ob[:, t4 * 128:(t4 + 1) * 128]
                    if has_prev(g, t):
                        pj = slot_of[(hd, t - 1)]
                        mm(oc_, V_sb[i][:, t - 1, hd * 128:(hd + 1) * 128], Pm[pj][:, 128:256], True, False,
                           [B_V[i], B_pm[pj]], [obb])
                        mm(oc_, lv, Pm[j][:, 0:128], False, True, [B_V[i], B_pm[j]], [obb])
                    else:
                        mm(oc_, lv, Pm[j][:, 0:128], True, True, [B_V[i], B_pm[j]], [obb])
                    if t4 == 3:
                        dst = unit_ap(acc[hd], g, u)
                        obv = ob.rearrange("p (a b) -> p a b", a=4)
                        if g == 0:
                            cp("dve", dst, obv, [obb], [B_acc[hd]])
                        else:
                            tt("dve", dst, obv, dst, ALU.add, [obb, B_acc[hd]], [B_acc[hd]])

                LA = 2
                for idx in range(len(items) + LA):
                    if idx < len(items):
                        stage1(*items[idx])
                    if idx >= LA:
                        stage2(*items[idx - LA])
            act(rtmp[0:64, :], acc[0][64:128, :], AF.Ln, [B_acc[0]], [B_rtmp])
            act(rtmp[0:64, :], rtmp[0:64, :], AF.Exp, [B_rtmp], [B_rtmp], scale=-1.0)
            tt("dve", attnT[0:64, sp, :], acc[0][0:64, :], rtmp[0:64, :], ALU.mult, [B_acc[0], B_rtmp], [B_attn[sp]])
            act(rtmp[64:128, :], acc[1][0:64, :], AF.Ln, [B_acc[1]], [B_rtmp])
            act(rtmp[64:128, :], rtmp[64:128, :], AF.Exp, [B_rtmp], [B_rtmp], scale=-1.0)
            tt("dve", attnT[64:128, sp, :], acc[1][64:128, :], rtmp[64:128, :], ALU.mult, [B_acc[1], B_rtmp], [B_attn[sp]])
        if b == 0:
            dump("attnT", attnT, [128, 4, SEQ], BF16, B_attn)

        mC = mark()
        S.tag = "C"
        wxr = [carve([128, 8, 128], BF16) for _ in range(2)]; B_wxr = [Buf(), Buf()]
        wyr = [carve([128, 8, 128], BF16) for _ in range(2)]; B_wyr = [Buf(), Buf()]
        NQ = 4
        HL = SEQ // NQ
        RS = [[carve([128, HL + 16], F32)] + [carve([128, HL], F32) for _ in range(3)] for _ in range(2)]
        B_RS = [[Buf() for _ in range(4)] for _ in range(2)]
        YS = [[carve([128, HL], F32) for _ in range(2)] for _ in range(2)]
        B_YS = [[Buf(), Buf()] for _ in range(2)]
        xcbs = [carve([128, HL], BF16) for _ in range(2)]; B_xcbs = [Buf(), Buf()]
        halo = carve([128, 4], F32); B_halo = Buf()
        un = 0
        for c in range(8):
            i = c % 2
            dma("pool", wxr[i], wcols(4608 + c * 128), [], [B_wxr[i]])
            dma("pool", wyr[i], wcols(5632 + c * 128), [], [B_wyr[i]])
            for hf in range(NQ):
                s = un % 2; un += 1
                R1, R2, R3, R4 = RS[s]; b1, b2, b3, b4 = B_RS[s]
                Y1, Y2 = YS[s]; by1, by2 = B_YS[s]
                xcb = xcbs[s]; bxc = B_xcbs[s]
                pR2 = RS[1 - s][1]; pb2 = B_RS[1 - s][1]
                if hf == 0:
                    S.op("pool", lambda R1=R1: nc.gpsimd.memset(R1[:, 0:3], 0.0), [], [b1])
                else:
                    cp("pool", R1[:, 0:3], halo[:, 0:3], [B_halo], [b1])
                for t2 in range(1):
                    tb = hf
                    ps, pb = bank()
                    for kc in range(8):
                        mm(ps, wxr[i][:, kc, :], hT[:, kc, tb * 512:(tb + 1) * 512], kc == 0, kc == 7, [B_wxr[i], B_hT[tb]], [pb])
                    cp("act", R1[:, 3 + t2 * 512:3 + (t2 + 1) * 512], ps, [pb], [b1])
                if hf < NQ - 1:
                    cp("pool", halo[:, 0:3], R1[:, HL:HL + 3], [b1], [B_halo])
                ts("dve", R2, R1[:, 3:3 + HL], convw_sb[:, c, 3:4], lruv_sb[:, 0, c:c + 1], ALU.mult, ALU.add,
                   [b1, B_lruc], [b2])
                for j in range(3):
                    stt(R2, R1[:, j:j + HL], convw_sb[:, c, j:j + 1], R2, ALU.mult, ALU.add, [b1, b2, B_lruc], [b2])
                cp("act", xcb, R2, [b2], [bxc])
                issue_conv(2, after=[bxc])
                for (wb, bias_ap, dst, bdst) in ((wxb_sb, lruv_sb[:, 1, c:c + 1], R3, b3), (wab_sb, lruv_sb[:, 2, c:c + 1], R4, b4)):
                    for t2 in range(1):
                        ps, pb = bank()
                        mm(ps, wb[:, c, :], xcb[:, t2 * 512:(t2 + 1) * 512], True, True, [B_lruc, bxc], [pb])
                        act(dst[:, t2 * 512:(t2 + 1) * 512], ps, AF.Sigmoid, [pb, B_lruc], [bdst], bias=bias_ap)
                act(R1[:, 0:HL], R4, AF.Exp, [b4, B_lruc], [b1], scale=cl_sb[:, 1, c:c + 1])
                act(R4, R4, AF.Exp, [b4, B_lruc], [b4], scale=cl_sb[:, 2, c:c + 1])
                act(R4, R4, AF.Ln, [b4, B_const], [b4], bias=one_ap, scale=-1.0)
                act(R4, R4, AF.Exp, [b4], [b4], scale=0.5)
                tt("pool", R3, R3, R2, ALU.mult, [b3, b2], [b3])
                tt("pool", R3, R3, R4, ALU.mult, [b3, b4], [b3])
                if hf == 0:
                    S.op("dve", lambda R1=R1, R2=R2, R3=R3: nc.vector.tensor_tensor_scan(
                        out=R2, data0=R1[:, 0:HL], data1=R3, initial=0.0, op0=ALU.mult, op1=ALU.add), [b1, b3], [b2], cost=1.3)
                else:
                    S.op("dve", lambda R1=R1, R2=R2, R3=R3, pR2=pR2: nc.vector.tensor_tensor_scan(
                        out=R2, data0=R1[:, 0:HL], data1=R3, initial=pR2[:, HL - 1:HL], op0=ALU.mult, op1=ALU.add),
                        [b1, b3, pb2], [b2], cost=1.3)
                for t2 in range(1):
                    tb = hf
                    ps, pb = bank()
                    for kc in range(8):
                        mm(ps, wyr[i][:, kc, :], hT[:, kc, tb * 512:(tb + 1) * 512], kc == 0, kc == 7, [B_wyr[i], B_hT[tb]], [pb])
                    cp("act", Y1[:, t2 * 512:(t2 + 1) * 512], ps, [pb], [by1])
                    act(Y2[:, t2 * 512:(t2 + 1) * 512], ps, AF.Square, [pb], [by2])
                ts("dve", Y2, Y2, 0.044715, 1.0, ALU.mult, ALU.add, [by2], [by2])
                tt("pool", Y2, Y2, Y1, ALU.mult, [by2, by1], [by2])
                act(Y2, Y2, AF.Sigmoid, [by2], [by2], scale=1.5957691216057308)
                tt("pool", Y2, Y2, Y1, ALU.mult, [by2, by1], [by2])
                tt("dve", lruT[:, c, hf * HL:(hf + 1) * HL], Y2, R2, ALU.mult, [by2, b2], [B_lru[c]])
        if b == 0:
            dump("lruT", lruT, [128, 8, SEQ], BF16, B_lru)
        S.tag = "m"
        release(mB)
        if stage < 4:
            release(mseq)
            return

        mix_off = mark()
        mixedT = carve([128, 8, SEQ], BF16); B_mix = [Buf("mix%d" % i) for i in range(4)]
        mD1 = mark()
        wA = [carve([128, 4, 128], BF16) for _ in range(2)]; B_wA = [Buf(), Buf()]
        wB = [carve([128, 8, 128], BF16) for _ in range(2)]; B_wB = [Buf(), Buf()]
        wgA = [carve([128, 8, 128], BF16) for _ in range(2)]; B_wgA = [Buf(), Buf()]
        wgB = [carve([128, 8, 128], BF16) for _ in range(2)]; B_wgB = [Buf(), Buf()]
        sg = [carve([128, 512], F32) for _ in range(4)]; B_sg = [Buf() for _ in range(4)]
        tm = [carve([128, 512], F32) for _ in range(4)]; B_tm = [Buf() for _ in range(4)]
        kk = 0
        for oc in range(8):
            i = oc % 2
            if b == 0:
                for zb in range(12):
                    blk_ = oc * 12 + zb
                    dma("sp", xp_d[blk_ * 128:(blk_ + 1) * 128, :].rearrange("r (a n) -> r a n", a=8), zeros_b8, [B_const], [])
            dma("pool", wA[i], w_attn_o[:, oc * 128:(oc + 1) * 128].rearrange("(c p) n -> p c n", p=128), [], [B_wA[i]])
            dma("pool", wB[i], w_lru_o[:, oc * 128:(oc + 1) * 128].rearrange("(c p) n -> p c n", p=128), [], [B_wB[i]])
            dma("pool", wgA[i], wcols(6656 + oc * 128), [], [B_wgA[i]])
            dma("pool", wgB[i], wcols(7680 + oc * 128), [], [B_wgB[i]])
            for tb in range(4):
                tsl = slice(tb * 512, (tb + 1) * 512)
                pA, pAb = bank()
                for kc in range(4):
                    mm(pA, wA[i][:, kc, :], attnT[:, kc, tsl], kc == 0, kc == 3, [B_wA[i], B_attn[kc]], [pAb])
                pB, pBb = bank()
                for kc in range(8):
                    mm(pB, wB[i][:, kc, :], lruT[:, kc, tsl], kc == 0, kc == 7, [B_wB[i], B_lru[kc]], [pBb])
                pGA, pGAb = bank()
                for kc in range(8):
                    mm(pGA, wgA[i][:, kc, :], hT[:, kc, tsl], kc == 0, kc == 7, [B_wgA[i], B_hT[tb]], [pGAb])
                pGB, pGBb = bank()
                for kc in range(8):
                    mm(pGB, wgB[i][:, kc, :], hT[:, kc, tsl], kc == 0, kc == 7, [B_wgB[i], B_hT[tb]], [pGBb])
                j0 = kk % 4; j1 = (kk + 1) % 4; kk += 2
                act(sg[j0], pGA, AF.Sigmoid, [pGAb], [B_sg[j0]])
                act(sg[j1], pGB, AF.Sigmoid, [pGBb], [B_sg[j1]])
                tt("dve", tm[j0], pA, sg[j0], ALU.mult, [pAb, B_sg[j0]], [B_tm[j0]])
                tt("dve", tm[j1], pB, sg[j1], ALU.mult, [pBb, B_sg[j1]], [B_tm[j1]])
                tt("pool", mixedT[:, oc, tsl], tm[j0], tm[j1], ALU.add, [B_tm[j0], B_tm[j1]], [B_mix[tb]])
        if b == 0:
            dump("mixedT", mixedT, [128, 8, SEQ], BF16, B_mix)
        release(mseq)
        phase_d2(b, mixedT, B_mix)
        assert K.d2_top <= mix_off, (K.d2_top, mix_off)
        release(mseq)

    def phase_d2(b, mixedT, B_mix):
        mD2 = mark()
        g1f_rep = carve([128, D], F32); B_g1f = Buf()
        A2_rep = carve([128, D], F32); B_A2r = Buf()
        B2_rep = carve([128, D], F32); B_B2r = Buf()
        replicate(g1f_rep, B_g1f, lambda c: der[:, 2, c, b:b + 1])
        replicate(A2_rep, B_A2r, lambda c: der[:, 1, c, b:b + 1])
        replicate(B2_rep, B_B2r, lambda c: modT[:, 24 + c, b:b + 1])
        wout = carve([128, 8, D], BF16); B_wout = Buf()
        xt = [carve([128, D], F32) for _ in range(2)]; B_xt = [Buf(), Buf()]
        x1 = [carve([128, D], F32) for _ in range(2)]; B_x1 = [Buf(), Buf()]
        xn2_ = [carve([128, D], F32) for _ in range(2)]; B_xn2_ = [Buf(), Buf()]
        h2f_0 = carve([128, D], F32); h2f_ = [h2f_0, h2f_0]; B_h2f_0 = Buf(); B_h2f_ = [B_h2f_0, B_h2f_0]
        h2b = [carve([128, D], BF16) for _ in range(2)]; B_h2b = [Buf(), Buf()]
        h2T_ = [carve([128, 8, 128], F32) for _ in range(2)]; B_h2T_ = [Buf(), Buf()]
        junk = carve([128, D], BF16); B_junk = Buf()
        sq = [carve([128, 4], F32) for _ in range(2)]; B_sq = [Buf(), Buf()]
        lg_all = carve([128, NT, 36], F32); B_lgall = Buf()
        dma("pool", wout, w_out.rearrange("(c p) n -> p c n", p=128), [], [B_wout])
        for ti in range(NT):
            T = b * NT + ti
            i = ti % 2
            xn2 = xn2_[i]; B_xn2 = B_xn2_[i]; h2f = h2f_[i]; B_h2f = B_h2f_[i]
            h2T = h2T_[i]; B_h2T = B_h2T_[i]
            tsl = slice(ti * 128, (ti + 1) * 128)
            if ti == 0:
                dma("sp", xt[0], x[b, 0:128, :], [], [B_xt[0]])
            if ti + 1 < NT:
                dma("sp", xt[(ti + 1) % 2], x[b, (ti + 1) * 128:(ti + 2) * 128, :], [], [B_xt[(ti + 1) % 2]])
            for half in range(2):
                hs = slice(half * 512, (half + 1) * 512)
                ps, pb = bank()
                for kc in range(8):
                    mm(ps, mixedT[:, kc, tsl], wout[:, kc, hs], kc == 0, kc == 7, [B_mix[ti // 4], B_wout], [pb])
                tt("dve", x1[i][:, hs], ps, g1f_rep[:, hs], ALU.mult, [pb, B_g1f], [B_x1[i]])
            tt("pool", x1[i], x1[i], xt[i], ALU.add, [B_x1[i], B_xt[i]], [B_x1[i]])
            dma("sp", x1_d[T * 128:(T + 1) * 128, :], x1[i], [B_x1[i]], [])
            rms_stats(x1[i], B_x1[i], junk, B_junk, sq[i], B_sq[i])
            act(xn2, x1[i], AF.Copy, [B_x1[i], B_sq[i]], [B_xn2], scale=sq[i][:, 2:3])
            tt("dve", h2f, xn2, A2_rep, ALU.mult, [B_xn2, B_A2r], [B_h2f])
            tt("pool", h2b[i], h2f, B2_rep, ALU.add, [B_h2f, B_B2r], [B_h2b[i]])
            dma("sp", h2_d[T * 128:(T + 1) * 128, :], h2b[i], [B_h2b[i]], [])
            for half in range(2):
                ps, pb = bank()
                for c4 in range(4):
                    c = half * 4 + c4
                    tr(ps[:, c4 * 128:(c4 + 1) * 128], xn2[:, c * 128:(c + 1) * 128], ident_f, [B_xn2, B_const], [pb])
                for c4 in range(4):
                    c = half * 4 + c4
                    act(h2T[:, c, :], ps[:, c4 * 128:(c4 + 1) * 128], AF.Identity, [pb, B_mod], [B_h2T],
                        bias=modT[:, 24 + c, b:b + 1], scale=der[:, 1, c, b:b + 1])
            ps, pb = bank()
            for c in range(8):
                mm(ps[:, 0:36], h2T[:, c, :], wr_sb[:, c, :], c == 0, c == 7, [B_h2T, B_wr], [pb])
            tt("dve", lg_all[:, ti, :], ps[:, 0:36], br_sb, ALU.add, [pb, B_wr], [B_lgall])
        routing_seq(b, lg_all, B_lgall)
        if b == 0:
            dump("gw", gw_all, [128, 2, TT], F32, B_rt)
            dump("A0", A0_all, [128, TT, 32], BF16, B_rt)
            dump("A1", A1_all, [128, TT, 32], BF16, B_rt)
        K.d2_top = max(mark(), K.rt_top)
        release(mD2)

    def routing_seq(b, L, B_L):
        mR = mark()
        n = NT
        T0 = b * NT
        B_s = Buf("rscratch")
        R = [B_L, B_s]; W = [B_s]
        gl = L[:, :, 0:4]
        el = L[:, :, 4:36].rearrange("p t (g j) -> p t g j", g=4)
        gmax = carve([128, n], F32); goh = carve([128, n, 4], F32); gex = carve([128, n, 4], F32)
        gsum = carve([128, n], F32); ggate = carve([128, n], F32)
        tmp4 = carve([128, n, 4, 8], F32); ing = carve([128, n, 8], F32); msk = carve([128, n, 8], F32)
        m1 = carve([128, n], F32); m2 = carve([128, n], F32); oh0 = carve([128, n, 8], F32); oh1 = carve([128, n, 8], F32)
        e1 = carve([128, n], F32); rr_ = carve([128, n], F32)

        def bc3(a, k):
            return a.unsqueeze(2).to_broadcast([128, n, k])

        def red(out_, in_, op):
            S.op("dve", lambda: nc.vector.tensor_reduce(out=out_, in_=in_, axis=AX.X, op=op), R, W)

        red(gmax, gl, ALU.max)
        tt("dve", goh, gl, bc3(gmax, 4), ALU.is_equal, R, W)
        tt("dve", gex, gl, bc3(gmax, 4), ALU.subtract, R, W)
        act(gex, gex, AF.Exp, R, W)
        red(gsum, gex, ALU.add)
        S.op("dve", lambda: nc.vector.reciprocal(out=ggate, in_=gsum), R, W)
        tt("dve", tmp4, el, goh.unsqueeze(3).to_broadcast([128, n, 4, 8]), ALU.mult, R, W)
        red(ing, tmp4.rearrange("p t g j -> p t j g"), ALU.add)
        red(m1, ing, ALU.max)
        tt("dve", oh0, ing, bc3(m1, 8), ALU.is_equal, R, W)
        stt(msk, oh0, -1.0e30, ing, ALU.mult, ALU.add, R, W)
        red(m2, msk, ALU.max)
        tt("dve", oh1, msk, bc3(m2, 8), ALU.is_equal, R, W)
        tt("dve", e1, m2, m1, ALU.subtract, R, W)
        act(e1, e1, AF.Exp, R, W)
        ts("dve", rr_, e1, 1.0, None, ALU.add, None, R, W)
        S.op("dve", lambda: nc.vector.reciprocal(out=rr_, in_=rr_), R, W)
        Brt = [B_rt[T0 + t] for t in range(n)]
        W2 = [B_s] + Brt
        tt("dve", gw_all[:, 0, T0:T0 + n], rr_, ggate, ALU.mult, R, W2)
        tt("dve", e1, e1, rr_, ALU.mult, R, W)
        tt("dve", gw_all[:, 1, T0:T0 + n], e1, ggate, ALU.mult, R, W2)
        A0v = A0_all[:, T0:T0 + n, :].rearrange("p t (g j) -> p t g j", g=4)
        A1v = A1_all[:, T0:T0 + n, :].rearrange("p t (g j) -> p t g j", g=4)
        gohb = goh.unsqueeze(3).to_broadcast([128, n, 4, 8])
        tt("dve", A0v, oh0.unsqueeze(2).to_broadcast([128, n, 4, 8]), gohb, ALU.mult, R, W2)
        tt("dve", A1v, oh1.unsqueeze(2).to_broadcast([128, n, 4, 8]), gohb, ALU.mult, R, W2)
        tt("dve", As_all[:, T0:T0 + n, :], A0_all[:, T0:T0 + n, :], A1_all[:, T0:T0 + n, :], ALU.add, R + Brt, W2)
        K.rt_top = mark()
        release(mR)

    def phase_moe():
        issue_conv(len(conv_jobs))
        mM = mark()
        sz = carve([128, 32], F32); szi = carve([128, 32], I32); padf = carve([128, 32], F32)
        pend = carve([128, 32], F32); pstart = carve([128, 32], F32); ones32 = carve([128, 32], F32)
        B_sz = Buf()
        val = carve([128, 32], F32); tmpv = carve([128, 32], F32); dsum = carve([128, 2], F32); B_val = Buf()
        thr = carve([128, NBLK], F32); cmp3 = carve([128, NBLK, 32], F32); be_f = carve([128, NBLK], F32)
        idx_blk = carve([128, NBLK], U32); B_be = Buf()
        thr_i = carve([128, NBLK], I32)
        ps, pb = bank()
        for T in range(TT):
            mm(ps[:, 0:32], ones_b, As_all[:, T, :], T == 0, T == TT - 1, [B_const, B_rt[T]], [pb])
        ts("dve", szi, ps[:, 0:32], 127.0, None, ALU.add, None, [pb], [B_sz])
        S.op("dve", lambda: nc.vector.tensor_scalar(out=szi, in0=szi, scalar1=7, scalar2=7, op0=ALU.logical_shift_right,
                                                    op1=ALU.logical_shift_left), [B_sz], [B_sz])
        cp("dve", padf, szi, [B_sz], [B_sz])
        S.op("dve", lambda: nc.vector.memset(ones32, 1.0), [], [B_sz])
        S.op("dve", lambda: nc.vector.tensor_tensor_scan(out=pend, data0=ones32, data1=padf, initial=0.0,
                                                         op0=ALU.mult, op1=ALU.add), [B_sz], [B_sz])
        tt("dve", pstart, pend, padf, ALU.subtract, [B_sz], [B_sz])
        S.op("pool", lambda: nc.gpsimd.iota(thr_i, pattern=[[128, NBLK]], base=0, channel_multiplier=0), [], [B_be])
        cp("dve", thr, thr_i, [B_be], [B_be])
        tt("dve", cmp3, pend.unsqueeze(1).to_broadcast([128, NBLK, 32]), thr.unsqueeze(2).to_broadcast([128, NBLK, 32]),
           ALU.is_le, [B_sz, B_be], [B_be])
        S.op("dve", lambda: nc.vector.tensor_reduce(out=be_f, in_=cmp3, axis=AX.X, op=ALU.add), [B_be], [B_be])
        ts("dve", be_f, be_f, 31.0, 128.0, ALU.min, ALU.mult, [B_be], [B_be])
        skipm = carve([128, NBLK], F32)
        S.op("dve", lambda: nc.vector.memset(skipm, 0.0), [], [B_be])
        tt("dve", skipm[:, 1:NBLK], be_f[:, 1:NBLK], be_f[:, 0:NBLK - 1], ALU.is_equal, [B_be], [B_be])
        S.op("dve", lambda: nc.vector.memset(skipm[:, NBLK // 2:NBLK // 2 + 1], 0.0), [B_be], [B_be])
        stt(be_f, skipm, 1.0e6, be_f, ALU.mult, ALU.add, [B_be], [B_be])
        ts("dve", idx_blk, be_f, cst[:, 2:3], None, ALU.add, None, [B_be, B_const], [B_be])
        dump("pend", pend, [128, 32], F32, [B_sz])
        dump("idx_blk", idx_blk, [128, NBLK], U32, [B_be])
        h2l = [carve([128, D], BF16) for _ in range(6)]; B_h2l = [Buf() for _ in range(6)]
        B_xp = Buf("xp")
        for T in range(TT):
            ps, pb = bank()
            mm(ps[:, 0:32], ustrict, As_all[:, T, :], True, T == 0, [B_const, B_rt[T]], [pb])
            for T2 in range(T):
                mm(ps[:, 0:32], ones_b, As_all[:, T2, :], False, T2 == T - 1, [B_const, B_rt[T2]], [pb])
            tt("dve", val, ps[:, 0:32], pstart, ALU.add, [pb, B_sz], [B_val])
            for kx, A_ in enumerate((A0_all, A1_all)):
                tt("dve", tmpv, val, A_[:, T, :], ALU.mult, [B_val, B_rt[T]], [B_val])
                S.op("dve", lambda kx=kx: nc.vector.reduce_sum(out=dsum[:, kx:kx + 1], in_=tmpv, axis=AX.X), [B_val], [B_val])
                cp("dve", dest_all[:, kx, T:T + 1], dsum[:, kx:kx + 1], [B_val], [B_dest[T]])
            j = T % 6
            dma("sp", h2l[j], h2_d[T * 128:(T + 1) * 128, :], [], [B_h2l[j]])
            for kx in range(2):
                if "noscat" in dbg or ("scat1" in dbg and T >= 1):
                    continue
                scatter(xp_d, dest_all[:, kx, T:T + 1], h2l[j], [B_h2l[j], B_dest[T]], [])
        dump("dest", dest_all, [128, 2, TT], U32, B_dest)
        S.barrier()
        if stage < 6:
            release(mM)
            return
        mE = mark()
        w1f = [carve([128, 4096], BF16) for _ in range(2)]; B_w1 = [Buf(), Buf()]
        w3f = [carve([128, 4096], BF16) for _ in range(2)]; B_w3 = [Buf(), Buf()]
        w2f = [carve([128, 4096], BF16) for _ in range(2)]; B_w2 = [Buf(), Buf()]
        w1s = [t.rearrange("p (c n) -> p c n", c=8) for t in w1f]
        w3s = [t.rearrange("p (c n) -> p c n", c=8) for t in w3f]
        w2s = [t.rearrange("p (c n) -> p c n", c=4) for t in w2f]
        XB = 3
        xb = [carve([128, D], BF16) for _ in range(XB)]; B_xb = [Buf() for _ in range(XB)]
        xT = [carve([128, 8, 128], BF16) for _ in range(2)]; B_xT = [Buf(), Buf()]
        s1 = [carve([128, 512], F32) for _ in range(2)]; B_s1 = [Buf(), Buf()]
        gTt = [carve([128, 4, 128], BF16) for _ in range(2)]; B_gT = [Buf(), Buf()]
        ysb = [carve([128, D], F32) for _ in range(2)]; B_ys = [Buf(), Buf()]
        B_yp = Buf("yp")

        HB = NBLK // 2
        border = [(k // 2) if k % 2 == 0 else HB + k // 2 for k in range(NBLK)]

        def gather_w(out_, in_, idx, R, W):
            def fn():
                if getattr(K, "breg", None) is None:
                    K.breg = nc.gpsimd.to_reg(4095)
                return nc.gpsimd.indirect_dma_start(
                    out=out_, out_offset=None, in_=in_, in_offset=bass.IndirectOffsetOnAxis(ap=idx, axis=0),
                    bounds_check=K.breg, oob_is_err=False)
            return S.dma("pool", fn, R, W, xfer=9.0)

        def load_x(blk):
            j = blk % XB
            bid = border[blk]
            dma("sp", xb[j], xp_d[bid * 128:(bid + 1) * 128, :], [B_xp], [B_xb[j]])

        def load_w13(blk):
            i = blk % 2
            bid = border[blk]
            idx = idx_blk[:, bid:bid + 1]
            gather_w(w1f[i], w1b, idx, [B_be], [B_w1[i]])
            gather_w(w3f[i], w3b, idx, [B_be], [B_w3[i]])

        def load_w2(blk):
            i = blk % 2
            bid = border[blk]
            idx = idx_blk[:, bid:bid + 1]
            gather_w(w2f[i], w2b, idx, [B_be], [B_w2[i]])

        def stageA(blk):
            i = blk % 2
            j = blk % XB
            ps, pb = bank()
            psb = ps.bitcast(BF16)
            xbv = xb[j].rearrange("r (p c) -> r c p", c=8)
            for c in range(8):
                tr(psb[:, c * 128:(c + 1) * 128], xbv[:, c, :], ident_b, [B_xb[j], B_const], [pb])
            cp("dve", xT[i].rearrange("p c r -> p (c r)"), psb, [pb], [B_xT[i]])
            p1, p1b = bank()
            p3, p3b = bank()
            for (pp, ppb, ws, bws) in ((p1, p1b, w1s[i], B_w1[i]), (p3, p3b, w3s[i], B_w3[i])):
                wsv = ws.rearrange("p c (m j) -> p c j m", j=4)
                for jj in range(4):
                    for c in range(8):
                        mm(pp[:, jj * 128:(jj + 1) * 128], wsv[:, c, jj, :], xT[i][:, c, :], c == 0, c == 7, [bws, B_xT[i]], [ppb])
            act(s1[i], p1, AF.Sigmoid, [p1b], [B_s1[i]])
            tt("dve", s1[i], s1[i], p1, ALU.mult, [B_s1[i], p1b], [B_s1[i]])
            tt("dve", gTt[i].rearrange("p j r -> p (j r)"), s1[i], p3, ALU.mult, [B_s1[i], p3b], [B_gT[i]])

        def stageB(blk):
            i = blk % 2
            bid = border[blk]
            for half in range(2):
                py, pyb = bank()
                for jj in range(4):
                    mm(py, gTt[i][:, jj, :], w2s[i][:, jj, half * 512:(half + 1) * 512], jj == 0, jj == 3, [B_gT[i], B_w2[i]], [pyb])
                if half == 0:
                    cp("act", ysb[i][:, 0:512], py, [pyb], [B_ys[i]])
                else:
                    cp("dve", ysb[i][:, 512:1024], py, [pyb], [B_ys[i]])
            dma("sp", yp_d[bid * 128:(bid + 1) * 128, :], ysb[i], [B_ys[i]], [])

        load_x(0); load_x(1)
        load_w13(0); load_w13(1); load_w2(0); load_w2(1)
        for blk in range(NBLK + 1):
            if blk < NBLK:
                if blk + 2 < NBLK:
                    load_x(blk + 2)
                stageA(blk)
                if blk + 2 < NBLK:
                    load_w13(blk + 2)
            if blk >= 1:
                stageB(blk - 1)
                if 2 <= blk + 1 < NBLK:
                    load_w2(blk + 1)
        S.barrier()
        release(mE)
        if stage < 7:
            release(mM)
            return
        NB3 = 5
        g2f_rep = carve([128, NS, D], F32); B_g2f = Buf()
        gf_sb = carve([128, D], F32); B_gf = Buf()
        dma("sp", gf_sb, gf_rep, [], [B_gf])
        for b_ in range(NS):
            replicate(g2f_rep[:, b_, :], B_g2f, lambda c, b_=b_: der[:, 3, c, b_:b_ + 1])
        x1l = [carve([128, D], F32) for _ in range(NB3)]; B_x1l = [Buf() for _ in range(NB3)]
        y0 = [carve([128, D], F32) for _ in range(NB3)]; B_y0 = [Buf() for _ in range(NB3)]
        y1 = [carve([128, D], F32) for _ in range(NB3)]; B_y1 = [Buf() for _ in range(NB3)]
        ot = [carve([128, D], F32) for _ in range(2)]; B_ot = [Buf(), Buf()]
        junk = carve([128, D], BF16); B_junk = Buf()
        sq = [carve([128, 4], F32) for _ in range(2)]; B_sq = [Buf(), Buf()]

        def comb_load(T):
            i = T % NB3
            dma("sp", x1l[i], x1_d[T * 128:(T + 1) * 128, :], [], [B_x1l[i]])
            gather(y0[i], yp_d, dest_all[:, 0, T:T + 1], [B_yp, B_dest[T]], [B_y0[i]])
            gather(y1[i], yp_d, dest_all[:, 1, T:T + 1], [B_yp, B_dest[T]], [B_y1[i]])

        comb_load(0); comb_load(1)
        for T in range(TT):
            b, ti = divmod(T, NT)
            i = T % NB3
            k2 = T % 2
            if T + 2 < TT:
                comb_load(T + 2)
            act(y0[i], y0[i], AF.Copy, [B_y0[i], B_rt[T]], [B_y0[i]], scale=gw_all[:, 0, T:T + 1])
            stt(y0[i], y1[i], gw_all[:, 1, T:T + 1], y0[i], ALU.mult, ALU.add, [B_y0[i], B_y1[i], B_rt[T]], [B_y0[i]])
            tt("dve", y0[i], y0[i], g2f_rep[:, b, :], ALU.mult, [B_y0[i], B_g2f], [B_y0[i]])
            tt("pool", y0[i], y0[i], x1l[i], ALU.add, [B_y0[i], B_x1l[i]], [B_y0[i]])
            rms_stats(y0[i], B_y0[i], junk, B_junk, sq[k2], B_sq[k2])
            act(y0[i], y0[i], AF.Copy, [B_y0[i], B_sq[k2]], [B_y0[i]], scale=sq[k2][:, 2:3])
            tt("dve", ot[k2], y0[i], gf_sb, ALU.mult, [B_y0[i], B_gf], [B_ot[k2]])
            dma("sp", out[b, ti * 128:(ti + 1) * 128, :], ot[k2], [B_ot[k2]], [])
        release(mM)


    K.last = None
    if stage >= 1:
        for b in range(NS):
            phase_seq(b)
    if stage >= 5:
        phase_moe()

    S.replay()
    return nc, dbg_out


def _fm(v):
    return np.ascontiguousarray(np.asarray(v).reshape(8, 128).T)


def _shared_inputs(inp):
    f = lambda a: np.ascontiguousarray(np.asarray(a, dtype=np.float32))
    sh = {}
    sh["w_mod"] = f(inp["w_mod"][0])
    sh["b_modT"] = f(np.asarray(inp["b_mod"][0]).reshape(48, 128).T)
    sh["gT"] = f(np.stack([_fm(inp["norm1_g"][0]), _fm(inp["norm2_g"][0])], axis=1))
    sh["gf_rep"] = f(np.broadcast_to(np.asarray(inp["norm_f_g"])[None, :], (128, D)))
    sh["w_in"] = f(inp["w_in"][0])
    sh["conv_wT"] = f(np.asarray(inp["conv_w"][0]).T.reshape(8, 128, 4).transpose(1, 0, 2))
    vecs = np.stack([np.asarray(inp["conv_b"][0]), np.asarray(inp["lru_bx"][0]),
                     np.asarray(inp["lru_ba"][0]), np.asarray(inp["lru_lambda"][0])], axis=0)
    sh["lru_vecs"] = f(vecs.reshape(4, 8, 128).transpose(2, 0, 1))
    for name, key in (("wx_blk", "lru_wx"), ("wa_blk", "lru_wa")):
        wsrc = np.asarray(inp[key][0])
        blk = np.zeros((8, 128, 128), np.float32)
        for n in range(16):
            o = (n % 2) * 64
            blk[n // 2, o:o + 64, o:o + 64] = wsrc[n]
        sh[name] = blk
    sh["w_attn_o"] = f(inp["w_attn_o"][0])
    sh["w_lru_o"] = f(inp["w_lru_o"][0])
    sh["w_out"] = f(inp["w_out"][0])
    wr_full = np.concatenate([np.asarray(inp["w_grp"][0]), np.asarray(inp["w_exp"][0])], axis=1)
    sh["wr"] = f(wr_full.reshape(8, 128, 36).transpose(1, 0, 2))
    br = np.concatenate([np.asarray(inp["b_grp"][0]), np.asarray(inp["b_exp"][0])], axis=0)
    sh["br_rep"] = f(np.broadcast_to(br[None, :], (128, 36)))
    sh["w1"] = f(np.asarray(inp["w1"][0]).reshape(4096, 4096))
    sh["w3"] = f(np.asarray(inp["w3"][0]).reshape(4096, 4096))
    sh["w2"] = f(np.asarray(inp["w2"][0]).reshape(4096, 4096))
    return sh


def _core_inputs(inp, sh, core):
    m = dict(sh)
    xs = np.asarray(inp["x"])[core * NS:(core + 1) * NS]
    m["x"] = np.ascontiguousarray(xs, dtype=np.float32)
    cs = np.asarray(inp["c"])[core * NS:(core + 1) * NS]
    m["cT"] = np.ascontiguousarray(cs.T.reshape(8, 128, NS).transpose(1, 0, 2), dtype=np.float32)
    return m


def kernel(**inputs):
    nc, _ = build_program()
    sh = _shared_inputs(inputs)
    in_maps = [_core_inputs(inputs, sh, core) for core in range(NCORES)]
    res = run_bass_kernel_spmd(nc, in_maps, core_ids=list(range(NCORES)))
    outs = [np.asarray(r["out"], dtype=np.float32) for r in res.results]
    return np.concatenate(outs, axis=0)
```
